# Optimizing a Trainium2 kernel written in Bass

```python
import jax, jax.numpy as jnp
from jax import lax
import numpy as np

D_MODEL = 2048
BATCH = 4
SEQ = 2048
DEPTH = 1

GRID_W = 64
MIX_WIDTH = D_MODEL
FOURIER_WIDTH = MIX_WIDTH // 4
FOURIER_GROUP_DIM = 128
FOURIER_GROUPS = FOURIER_WIDTH // FOURIER_GROUP_DIM
ATTN_WIDTH = MIX_WIDTH - FOURIER_WIDTH
HEAD_DIM = 128
N_Q_HEADS = ATTN_WIDTH // HEAD_DIM
GQA_GROUP = 3
N_KV_HEADS = N_Q_HEADS // GQA_GROUP
KV_WIDTH = N_KV_HEADS * HEAD_DIM
IN_WIDTH = FOURIER_WIDTH + ATTN_WIDTH + 2 * KV_WIDTH
ROPE_THETA = 10000.0
AXIS_ROPE_DIM = HEAD_DIM // 2
Q_BLOCK = 128
N_EXPERTS = 32
TOP_K = 4
D_FF_EXPERT = D_MODEL
SWIGLU_LIMIT = 7.0
SWIGLU_ALPHA = 1.702
EXPERT_BLOCK = 128
N_MOD = 6
EPS = 1e-6

kernel_name = 'hybrid_fnet_axialgqa_moe_block'


def rms_norm(x, g):
    xf = x.astype(jnp.float32)
    y = xf * lax.rsqrt(jnp.mean(xf * xf, axis=-1, keepdims=True) + EPS)
    return (y * g.astype(jnp.float32)).astype(x.dtype)


def modulate(h, shift, scale):
    return h * (1.0 + scale[:, None, :]) + shift[:, None, :]


def axial_rope_tables(seq_len):
    rows = seq_len // GRID_W
    row_idx = jnp.repeat(jnp.arange(rows, dtype=jnp.float32), GRID_W)
    col_idx = jnp.tile(jnp.arange(GRID_W, dtype=jnp.float32), rows)
    inv_freq = 1.0 / (ROPE_THETA ** (jnp.arange(0, AXIS_ROPE_DIM, 2, dtype=jnp.float32) / AXIS_ROPE_DIM))
    ang = jnp.concatenate([row_idx[:, None] * inv_freq, col_idx[:, None] * inv_freq], axis=-1)
    return jnp.cos(ang), jnp.sin(ang)


def apply_rope(x, cos, sin):
    xf = x.astype(jnp.float32).reshape(x.shape[:-1] + (HEAD_DIM // 2, 2))
    x0, x1 = xf[..., 0], xf[..., 1]
    c = cos[None, :, None, :]
    s = sin[None, :, None, :]
    out = jnp.stack([x0 * c - x1 * s, x0 * s + x1 * c], axis=-1).reshape(x.shape)
    return out.astype(x.dtype)


def fourier_mix(f, w_fourier):
    b, s, _ = f.shape
    fu = f.reshape(b, s, FOURIER_GROUPS, FOURIER_GROUP_DIM).astype(jnp.float32)
    fm = jnp.fft.fft2(fu, axes=(1, 3), norm='ortho').real.astype(f.dtype)
    fo = jnp.einsum('bsgc,gcd->bsgd', fm, w_fourier)
    return fo.reshape(b, s, FOURIER_WIDTH)


def block_attention(q, k, v):
    b, s = q.shape[0], q.shape[1]
    nb = s // Q_BLOCK
    qb = q.reshape(b, nb, Q_BLOCK, N_KV_HEADS, GQA_GROUP, HEAD_DIM).transpose(1, 0, 2, 3, 4, 5)
    scale = HEAD_DIM ** -0.5

    def one_block(q_blk):
        sc = jnp.einsum('bqkgd,bskd->bkgqs', q_blk, k).astype(jnp.float32) * scale
        p = jax.nn.softmax(sc, axis=-1).astype(v.dtype)
        return jnp.einsum('bkgqs,bskd->bqkgd', p, v)

    o = lax.map(one_block, qb)
    return o.transpose(1, 0, 2, 3, 4, 5).reshape(b, s, ATTN_WIDTH)


def clamped_swiglu(g, u):
    g = jnp.minimum(g, SWIGLU_LIMIT)
    u = jnp.clip(u, -SWIGLU_LIMIT, SWIGLU_LIMIT)
    return (u + 1.0) * (g * jax.nn.sigmoid(SWIGLU_ALPHA * g))


def moe_ffn(h, w_router, b_router, w_gate, b_gate, w_up, b_up, w_down, b_down):
    b, s, d = h.shape
    n_tok = b * s
    hf = h.reshape(n_tok, d)
    logits = (hf @ w_router).astype(jnp.float32) + b_router.astype(jnp.float32)
    top_val, top_idx = lax.top_k(logits, TOP_K)
    gates = jax.nn.softmax(top_val, axis=-1).astype(h.dtype)
    n_assign = n_tok * TOP_K
    flat_e = top_idx.reshape(-1).astype(jnp.int32)
    flat_tok = jnp.arange(n_assign, dtype=jnp.int32) // TOP_K
    flat_gate = gates.reshape(-1)
    order = jnp.argsort(flat_e)
    sorted_e = flat_e[order]
    counts = jnp.zeros((N_EXPERTS,), jnp.int32).at[flat_e].add(1)
    padded = (counts + EXPERT_BLOCK - 1) // EXPERT_BLOCK * EXPERT_BLOCK
    pad_end = jnp.cumsum(padded)
    pad_start = pad_end - padded
    start = jnp.cumsum(counts) - counts
    rank = jnp.arange(n_assign, dtype=jnp.int32) - start[sorted_e]
    dest = pad_start[sorted_e] + rank
    n_pad = n_assign + N_EXPERTS * EXPERT_BLOCK
    n_blk = n_pad // EXPERT_BLOCK
    row_tok = jnp.full((n_pad,), n_tok, jnp.int32).at[dest].set(flat_tok[order])
    row_gate = jnp.zeros((n_pad,), h.dtype).at[dest].set(flat_gate[order])
    blk_start = jnp.arange(n_blk, dtype=jnp.int32) * EXPERT_BLOCK
    blk_e = jnp.minimum(jnp.searchsorted(pad_end, blk_start, side='right'), N_EXPERTS - 1)
    x_pad = jnp.concatenate([hf, jnp.zeros((1, d), hf.dtype)], axis=0)

    def expert_block(args):
        tok, e = args
        xb = x_pad[tok]
        g = xb @ w_gate[e] + b_gate[e]
        u = xb @ w_up[e] + b_up[e]
        return clamped_swiglu(g, u) @ w_down[e] + b_down[e]

    out = lax.map(expert_block, (row_tok.reshape(n_blk, EXPERT_BLOCK), blk_e))
    out = out.reshape(n_pad, d) * row_gate[:, None]
    y = jnp.zeros((n_tok + 1, d), h.dtype).at[row_tok].add(out)[:n_tok]
    return y.reshape(b, s, d)


def setup_inputs(seed: int = 0) -> dict:
    key = jax.random.key(seed)
    ks = jax.random.split(key, 24)
    f32 = jnp.float32
    def nrm(k, shape, scale):
        return jax.random.normal(k, shape, f32) * scale
    def gain(k, shape):
        return 1.0 + 0.02 * jax.random.normal(k, shape, f32)
    L = DEPTH
    return {
        'x': nrm(ks[0], (BATCH, SEQ, D_MODEL), 1.0),
        'c': nrm(ks[1], (BATCH, D_MODEL), 1.0),
        'w_ada': nrm(ks[2], (L, D_MODEL, N_MOD * D_MODEL), 0.5 * D_MODEL ** -0.5),
        'b_ada': nrm(ks[3], (L, N_MOD * D_MODEL), 0.02),
        'g_pre_mix': gain(ks[4], (L, D_MODEL)),
        'w_in': nrm(ks[5], (L, D_MODEL, IN_WIDTH), D_MODEL ** -0.5),
        'w_fourier': nrm(ks[6], (L, FOURIER_GROUPS, FOURIER_GROUP_DIM, FOURIER_GROUP_DIM), FOURIER_GROUP_DIM ** -0.5),
        'q_norm_g': gain(ks[7], (L, HEAD_DIM)),
        'k_norm_g': gain(ks[8], (L, HEAD_DIM)),
        'g_fourier_out': gain(ks[9], (L, FOURIER_WIDTH)),
        'g_attn_out': gain(ks[10], (L, ATTN_WIDTH)),
        'w_out': nrm(ks[11], (L, MIX_WIDTH, D_MODEL), MIX_WIDTH ** -0.5),
        'g_post_mix': gain(ks[12], (L, D_MODEL)),
        'g_pre_ffn': gain(ks[13], (L, D_MODEL)),
        'w_router': nrm(ks[14], (L, D_MODEL, N_EXPERTS), D_MODEL ** -0.5),
        'b_router': nrm(ks[15], (L, N_EXPERTS), 0.01),
        'w_gate': nrm(ks[16], (L, N_EXPERTS, D_MODEL, D_FF_EXPERT), D_MODEL ** -0.5),
        'b_gate': nrm(ks[17], (L, N_EXPERTS, D_FF_EXPERT), 0.02),
        'w_up': nrm(ks[18], (L, N_EXPERTS, D_MODEL, D_FF_EXPERT), D_MODEL ** -0.5),
        'b_up': nrm(ks[19], (L, N_EXPERTS, D_FF_EXPERT), 0.02),
        'w_down': nrm(ks[20], (L, N_EXPERTS, D_FF_EXPERT, D_MODEL), D_FF_EXPERT ** -0.5),
        'b_down': nrm(ks[21], (L, N_EXPERTS, D_MODEL), 0.02),
        'g_post_ffn': gain(ks[22], (L, D_MODEL)),
    }


def reference(x, c, w_ada, b_ada, g_pre_mix, w_in, w_fourier, q_norm_g, k_norm_g, g_fourier_out, g_attn_out, w_out, g_post_mix, g_pre_ffn, w_router, b_router, w_gate, b_gate, w_up, b_up, w_down, b_down, g_post_ffn):
    b, s, _ = x.shape
    cos, sin = axial_rope_tables(s)
    for l in range(DEPTH):
        mod = (jax.nn.silu(c) @ w_ada[l] + b_ada[l]).reshape(b, N_MOD, D_MODEL)
        shift_m, scale_m, gate_m = mod[:, 0], mod[:, 1], mod[:, 2]
        shift_f, scale_f, gate_f = mod[:, 3], mod[:, 4], mod[:, 5]

        h = modulate(rms_norm(x, g_pre_mix[l]), shift_m, scale_m)
        proj = h @ w_in[l]
        f = proj[..., :FOURIER_WIDTH]
        q = proj[..., FOURIER_WIDTH:FOURIER_WIDTH + ATTN_WIDTH].reshape(b, s, N_Q_HEADS, HEAD_DIM)
        k = proj[..., FOURIER_WIDTH + ATTN_WIDTH:FOURIER_WIDTH + ATTN_WIDTH + KV_WIDTH].reshape(b, s, N_KV_HEADS, HEAD_DIM)
        v = proj[..., FOURIER_WIDTH + ATTN_WIDTH + KV_WIDTH:].reshape(b, s, N_KV_HEADS, HEAD_DIM)

        fo = fourier_mix(f, w_fourier[l])

        q = apply_rope(rms_norm(q, q_norm_g[l]), cos, sin)
        k = apply_rope(rms_norm(k, k_norm_g[l]), cos, sin)
        ao = block_attention(q, k, v)

        merged = jnp.concatenate([rms_norm(fo, g_fourier_out[l]), rms_norm(ao, g_attn_out[l])], axis=-1)
        mix = merged @ w_out[l]
        x = x + gate_m[:, None, :] * rms_norm(mix, g_post_mix[l])

        h = modulate(rms_norm(x, g_pre_ffn[l]), shift_f, scale_f)
        y = moe_ffn(h, w_router[l], b_router[l], w_gate[l], b_gate[l], w_up[l], b_up[l], w_down[l], b_down[l])
        x = x + gate_f[:, None, :] * rms_norm(y, g_post_ffn[l])
    return x
```

```python
import numpy as np
import ml_dtypes
from contextlib import ExitStack
import concourse.bass as bass
import concourse.mybir as mybir
from concourse.bass_utils import run_bass_kernel_spmd

F32 = mybir.dt.float32
BF16 = mybir.dt.bfloat16
ALU = mybir.AluOpType
AF = mybir.ActivationFunctionType
AX = mybir.AxisListType

P = 128
D = 2048
KC = 16
S = 2048
OWN = 1024
NCORE = 8
NE_OWN = 4
EPS = 1e-6
LIMIT = 7.0
ALPHA = 1.702


class Eng:
    def __init__(self, nc, eng, sem, serial):
        self.nc, self.eng, self.sem, self.n = nc, eng, sem, 0
        self.waited = {}
        self.serial = serial

    def after(self, *toks):
        for t in toks:
            if t is None:
                continue
            if t[0] == 'd':
                _, sem, val = t
                key = ('d', id(sem))
                if self.waited.get(key, 0) < val:
                    self.eng.wait_ge(sem, val)
                    self.waited[key] = val
            else:
                _, prod, n = t
                if prod is self and not self.serial:
                    continue
                key = ('e', id(prod))
                if self.waited.get(key, 0) < n:
                    self.eng.wait_ge(prod.sem, n)
                    self.waited[key] = n

    def done(self, instr):
        self.n += 1
        instr.then_inc(self.sem, 1)
        return ('e', self, self.n)

    def op(self, fn, *deps, ser=True):
        self.after(*deps)
        if self.serial and ser and self.n > 0:
            self.after(('e', self, self.n))
        return self.done(fn())


class Slot:
    def __init__(self, sem):
        self.sem, self.cnt = sem, 0

    def add(self, instr):
        self.cnt += 16
        instr.then_inc(self.sem, 16)
        return ('d', self.sem, self.cnt)

    def tok(self):
        return ('d', self.sem, self.cnt)


def build(stage=99):
    nc = bass.Bass("TRN2", target_bir_lowering=False)

    def din(name, shape, dt=F32):
        return nc.dram_tensor(name, list(shape), dt, kind="ExternalInput").ap()

    def dscr(name, shape, dt):
        return nc.dram_tensor(name, list(shape), dt, kind="Internal").ap()

    xp = din("xp", [S, D])
    c_col = din("c_col", [P, KC])
    w_ada = din("w_ada", [D, 6 * D])
    b_ada = din("b_ada", [1, 6 * D])
    gpm_col = din("gpm_col", [P, KC])
    gpf_col = din("gpf_col", [P, KC])
    w_in = din("w_in", [D, 3072])
    w_out = din("w_out", [D, D])
    wf = din("wf", [P, 4, P])
    qg_b = din("qg_b", [P, P])
    kg_b = din("kg_b", [P, P])
    gmerge_b = din("gmerge_b", [P, D])
    gpostm_b = din("gpostm_b", [P, D])
    gpostf_b = din("gpostf_b", [P, D])
    w_router = din("w_router", [P, KC, 32])
    b_router_b = din("b_router_b", [P, 32])
    wg = din("wg", [32, D, D])
    wu = din("wu", [32, D, D])
    wd = din("wd", [32, D, D])
    bg_col = din("bg_col", [P, 32, KC])
    bu_col = din("bu_col", [P, 32, KC])
    bd_b = din("bd_b", [32, P, D])
    cos_t = din("cos_t", [P, 16, 64])
    sin_t = din("sin_t", [P, 16, 64])
    ctab_f = din("ctab", [S, OWN])
    stab_f = din("stab", [S, OWN])
    cs_tab_f = din("cs_tab", [P, 256])
    ident_in = din("ident_in", [P, P])
    out = nc.dram_tensor("out", [OWN, D], F32, kind="ExternalOutput").ap()

    w_in_bf = dscr("w_in_bf", [D, 3072], BF16)
    ctab = dscr("ctab_bf", [S, OWN], BF16)
    stab = dscr("stab_bf", [S, OWN], BF16)
    cs_tab = dscr("cs_tab_bf", [P, 256], BF16)
    w_out_bf = dscr("w_out_bf", [D, D], BF16)
    x1_scr = dscr("x1_scr", [OWN, D], F32)
    ht_send = dscr("ht_send", [D, OWN], BF16)
    g_send = dscr("g_send", [OWN, 32], F32)
    gm_scr = dscr("gm_scr", [P, D], F32)
    gf_scr = dscr("gf_scr", [P, D], F32)

    with ExitStack() as top:
        nsem = [0]

        def newsem(name):
            nsem[0] += 1
            return top.enter_context(nc.semaphore(f"{name}_{nsem[0]}"))

        def slot(name="dq"):
            return Slot(newsem(name))

        pe = Eng(nc, nc.tensor, newsem("pe"), False)
        act = Eng(nc, nc.scalar, newsem("act"), True)
        dve = Eng(nc, nc.vector, newsem("dve"), True)
        pool = Eng(nc, nc.gpsimd, newsem("pool"), True)
        sp = Eng(nc, nc.sync, newsem("sp"), False)

        def sb(ctx, name, shape, dt):
            nsem[0] += 1
            return ctx.enter_context(nc.sbuf_tensor(f"{name}_{nsem[0]}", list(shape), dt))

        def ps(ctx, name, shape, dt=F32):
            nsem[0] += 1
            return ctx.enter_context(nc.psum_tensor(f"{name}_{nsem[0]}", list(shape), dt))

        def dma(q, out_ap, in_ap, sl, *deps):
            q.after(*deps)
            return sl.add(q.eng.dma_start(out=out_ap, in_=in_ap))

        def rstd(out_ap, in_ap, inv_n, *deps):
            t_a = act.op(lambda: nc.scalar.activation(out=out_ap, in_=in_ap, func=AF.Sqrt, scale=inv_n,
                                                      bias=eps_t[:, 0:1]), *deps)
            return dve.op(lambda: nc.vector.reciprocal(out=out_ap, in_=out_ap), t_a)

        def barrier(*extra):
            toks = [('e', pe, pe.n), ('e', act, act.n), ('e', dve, dve.n)] + list(extra)
            for en in (pe, act, dve, sp):
                en.after(*toks)

        cast_w_in = slot("cwi")
        cast_w_out = slot("cwo")
        for h in range(2):
            cast_w_in.add(nc.gpsimd.dma_start(out=w_in_bf[:, h * 1536:(h + 1) * 1536],
                                              in_=w_in[:, h * 1536:(h + 1) * 1536]))
        cast_tab = slot("ctb")
        cast_tab.add(nc.gpsimd.dma_start(out=cs_tab[:, :], in_=cs_tab_f[:, :]))
        cast_tab.add(nc.gpsimd.dma_start(out=ctab[:, :], in_=ctab_f[:, :]))
        cast_tab.add(nc.gpsimd.dma_start(out=stab[:, :], in_=stab_f[:, :]))
        cast_w_out.add(nc.gpsimd.dma_start(out=w_out_bf[:, :], in_=w_out[:, :]))

        ident_f = sb(top, "ident_f", [P, P], F32)
        ident_b = sb(top, "ident_b", [P, P], BF16)
        ones_f = sb(top, "ones_f", [P, P], F32)
        modcol = sb(top, "modcol", [P, 64], F32)
        a_m = sb(top, "a_m", [P, KC], F32)
        a_f = sb(top, "a_f", [P, KC], F32)
        t_id = dma(sp, ident_f[:], ident_in[:, :], slot("m"))
        t_idb = dve.op(lambda: nc.vector.tensor_copy(out=ident_b[:], in_=ident_f[:]), t_id)
        t_ones = dve.op(lambda: nc.vector.memset(ones_f[:], 1.0))
        eps_t = sb(top, "eps_t", [P, 1], F32)
        dve.op(lambda: nc.vector.memset(eps_t[:], EPS))

        with ExitStack() as ph:
            NWA = 3
            wa = [sb(ph, f"wa{i}", [P, KC, 512], F32) for i in range(NWA)]
            wa_sl = [slot("wa") for _ in range(NWA)]
            modrow = sb(ph, "modrow", [1, 6 * D], F32)
            cc = sb(ph, "cc", [P, KC], F32)
            sc = sb(ph, "sc", [P, KC], F32)
            gpm = sb(ph, "gpm", [P, KC], F32)
            gpf = sb(ph, "gpf", [P, KC], F32)
            gpb = sb(ph, "gpb", [P, D], F32)
            gm_row = sb(ph, "gm_row", [P, D], F32)
            gf_row = sb(ph, "gf_row", [P, D], F32)
            ps_row = [ps(ph, f"ps_row{i}", [P, 512]) for i in range(2)]
            ps_col = ps(ph, "ps_col", [P, 64])
            ps_bc = [ps(ph, f"ps_bc{i}", [P, 512]) for i in range(2)]
            misc = slot("m")
            t_c = dma(sp, cc[:], c_col[:, :], misc)
            t_c = dma(sp, modrow[:], b_ada[:, :], misc)
            t_c = dma(sp, gpm[:], gpm_col[:, :], misc)
            t_c = dma(sp, gpf[:], gpf_col[:, :], misc)
            t_sg = act.op(lambda: nc.scalar.activation(out=sc[:], in_=cc[:], func=AF.Sigmoid), t_c)
            t_sc = dve.op(lambda: nc.vector.tensor_tensor(out=sc[:], in0=sc[:], in1=cc[:], op=ALU.mult), t_sg)
            w_ada_v = w_ada.rearrange("(kc p) n -> p kc n", p=P)
            wa_free = [None] * NWA
            psr_free = [None, None]
            t_row = None
            t_lds = {}
            for nb in range(NWA - 1):
                t_lds[nb] = dma(sp, wa[nb % NWA][:], w_ada_v[:, :, nb * 512:(nb + 1) * 512], wa_sl[nb % NWA])
            for nb in range(24):
                b = nb % 2
                wb_ = nb % NWA
                nx = nb + NWA - 1
                if nx < 24:
                    t_lds[nx] = dma(sp, wa[nx % NWA][:], w_ada_v[:, :, nx * 512:(nx + 1) * 512], wa_sl[nx % NWA],
                                    wa_free[nx % NWA])
                pe.after(t_lds.pop(nb), t_sc, psr_free[b])
                for kc in range(KC):
                    mm = nc.tensor.matmul(ps_row[b][0:1, :], lhsT=sc[:, kc:kc + 1], rhs=wa[wb_][:, kc, :],
                                          start=(kc == 0), stop=(kc == KC - 1))
                t_mm = pe.done(mm)
                wa_free[wb_] = t_mm
                t_row = dve.op(lambda: nc.vector.tensor_tensor(
                    out=modrow[0:1, nb * 512:(nb + 1) * 512], in0=ps_row[b][0:1, :],
                    in1=modrow[0:1, nb * 512:(nb + 1) * 512], op=ALU.add), t_mm, t_c)
                psr_free[b] = t_row
            pe.after(t_row, t_ones)
            for v, j in enumerate((0, 1, 3, 4)):
                for c in range(KC):
                    mm = nc.tensor.matmul(ps_col[:, v * 16 + c:v * 16 + c + 1],
                                          lhsT=modrow[0:1, j * D + c * P:j * D + (c + 1) * P],
                                          rhs=ones_f[0:1, 0:1], start=True, stop=True)
            t_mm = pe.done(mm)
            t_mc = dve.op(lambda: nc.vector.tensor_copy(out=modcol[:], in_=ps_col[:]), t_mm)
            dve.op(lambda: nc.vector.scalar_tensor_tensor(out=a_m[:], in0=modcol[:, 16:32], scalar=1.0,
                                                          in1=gpm[:], op0=ALU.add, op1=ALU.mult))
            t_am = dve.op(lambda: nc.vector.scalar_tensor_tensor(out=a_f[:], in0=modcol[:, 48:64], scalar=1.0,
                                                                 in1=gpf[:], op0=ALU.add, op1=ALU.mult))
            bc_free = [None, None]
            k = 0
            for (j, dst, gsrc) in ((2, gm_row, gpostm_b), (5, gf_row, gpostf_b)):
                t_g = dma(sp, gpb[:], gsrc[:, :], slot("m"), t_am if k == 0 else t_bc)
                for nb in range(4):
                    b = k % 2
                    pe.after(bc_free[b])
                    mm = nc.tensor.matmul(ps_bc[b][:, :], lhsT=ones_f[0:1, :],
                                          rhs=modrow[0:1, j * D + nb * 512:j * D + (nb + 1) * 512],
                                          start=True, stop=True)
                    t_mm = pe.done(mm)
                    t_bc = dve.op(lambda: nc.vector.tensor_tensor(out=dst[:, nb * 512:(nb + 1) * 512],
                                                                  in0=ps_bc[b][:, :],
                                                                  in1=gpb[:, nb * 512:(nb + 1) * 512],
                                                                  op=ALU.mult), t_mm, t_g)
                    bc_free[b] = t_bc
                    k += 1
            g_sl = slot("gs")
            dma(sp, gm_scr[:, :], gm_row[:], g_sl, t_bc)
            t_gs = dma(sp, gf_scr[:, :], gf_row[:], g_sl)
            barrier(t_gs)

        with ExitStack() as mx:
            mergedT = sb(mx, "mergedT", [P, KC, OWN], BF16)
            cosb = sb(mx, "cosb", [P, 16, 64], F32)
            sinb = sb(mx, "sinb", [P, 16, 64], F32)
            qgb = sb(mx, "qgb", [P, P], F32)
            kgb = sb(mx, "kgb", [P, P], F32)
            kvq = mx.enter_context(ExitStack())
            kT = sb(kvq, "kT", [P, 4, S], BF16)
            vaug = sb(kvq, "vaug", [P, 16, 4, 132], BF16)
            qT = sb(kvq, "qT", [P, 4, 8, 384], BF16)
            misc = slot("m")
            t_tab = dma(sp, cosb[:], cos_t[:, :, :], misc)
            t_tab = dma(sp, sinb[:], sin_t[:, :, :], misc)
            t_tab = dma(sp, qgb[:], qg_b[:, :], misc)
            t_tab = dma(sp, kgb[:], kg_b[:, :], misc)
            t_v1 = dve.op(lambda: nc.vector.memset(vaug[:, :, :, 128:129], 1.0))

            with ExitStack() as zsc:
                zs = sb(zsc, "zs", [P, 16, 4, 256], BF16)
                cst = sb(zsc, "cst", [P, 256], BF16)
                t_cs = dma(sp, cst[:], cs_tab[:, :], slot("m"), cast_tab.tok())
                w_in_v = w_in_bf.rearrange("(kc p) n -> p kc n", p=P)
                for th in range(2):
                  with ExitStack() as pj:
                    hT = sb(pj, "hT", [P, KC, OWN], BF16)
                    with ExitStack() as ph:
                        xt = [sb(ph, f"xt{i}", [P, D], F32) for i in range(2)]
                        xt_sl = [slot("xt") for _ in range(2)]
                        xs = [sb(ph, f"xs{i}", [P, D], BF16) for i in range(2)]
                        junk = sb(ph, "junk", [P, D], F32)
                        ss = sb(ph, "ss", [P, 8], F32)
                        rs = sb(ph, "rs", [P, 8], F32)
                        dve.op(lambda: nc.vector.memset(ss[:], 0.0))
                        pst = [ps(ph, f"pst{i}", [P, 512], BF16) for i in range(4)]
                        xt_free = [None, None]
                        xs_free = [None, None]
                        pst_free = [None] * 4
                        kk = 0
                        t_ev = None
                        for i in range(8):
                            b = i % 2
                            tile = th * 8 + i
                            t_ld = dma(sp, xt[b][:], xp[tile * P:(tile + 1) * P, :], xt_sl[b], xt_free[b])
                            t_sq = act.op(lambda: nc.scalar.activation(out=junk[:], in_=xt[b][:], func=AF.Square,
                                                                       accum_out=ss[:, i:i + 1]), t_ld)
                            t_rs = rstd(rs[:, i:i + 1], ss[:, i:i + 1], 1.0 / D, t_sq)
                            t_xs = act.op(lambda: nc.scalar.activation(out=xs[b][:], in_=xt[b][:],
                                                                       func=AF.Identity, scale=rs[:, i:i + 1]),
                                          t_rs, xs_free[b])
                            xt_free[b] = t_xs
                            t_tr = None
                            for q4 in range(4):
                                pb = kk % 4
                                kk += 1
                                pe.after(t_xs, t_idb, pst_free[pb])
                                for c4 in range(4):
                                    dc = q4 * 4 + c4
                                    mm = nc.tensor.transpose(out=pst[pb][:, c4 * P:(c4 + 1) * P],
                                                             in_=xs[b][:, dc * P:(dc + 1) * P], identity=ident_b[:])
                                t_tr = pe.done(mm)
                                for c4 in range(4):
                                    dc = q4 * 4 + c4
                                    t_ev = act.op(lambda: nc.scalar.activation(
                                        out=hT[:, dc, i * P:(i + 1) * P], in_=pst[pb][:, c4 * P:(c4 + 1) * P],
                                        func=AF.Identity, scale=a_m[:, dc:dc + 1], bias=modcol[:, dc:dc + 1]), t_tr,
                                        ser=(c4 == 0))
                                pst_free[pb] = t_ev
                            xs_free[b] = t_tr
                        t_hT = t_ev
                    barrier()

                    with ExitStack() as ph:
                        wb = [sb(ph, f"wb{i}", [P, KC, 512], BF16) for i in range(1)]
                        wb_sl = [slot("wb") for _ in range(1)]
                        fT = sb(ph, "fT", [P, 4, OWN], BF16)
                        sq = sb(ph, "sq", [P, 512], F32)
                        t1 = sb(ph, "t1", [P, 512], F32)
                        m1 = sb(ph, "m1", [P, 256], F32)
                        m2 = sb(ph, "m2", [P, 256], F32)
                        qn = [sb(ph, f"qn{i}", [P, 512], BF16) for i in range(2)]
                        ssh = sb(ph, "ssh", [P, 4], F32)
                        rsh = sb(ph, "rsh", [P, 4], F32)
                        pp = [ps(ph, f"pp{i}", [P, 512]) for i in range(3)]
                        pz = [ps(ph, f"pz{i}", [P, 1024]) for i in range(1)]
                        ptq = [ps(ph, f"ptq{i}", [P, 512], BF16) for i in range(2)]
                        wb_free = [None, None]
                        pp_free = [None] * 3
                        ptq_free = [None, None]
                        qn_free = [None, None]
                        ppk = [0]
                        qk = [0]
                        order = [0, 4, 5] + ([1, 2, 3] if th == 0 else [])

                        def normrope(psrc, t_mm, gtile, tt, dst_bf, dst_free):
                            t_a = act.op(lambda: nc.scalar.activation(out=sq[:], in_=psrc[:, :], func=AF.Square), t_mm)
                            t_rd = dve.op(lambda: nc.vector.tensor_reduce(out=ssh[:], in_=sq[:].rearrange("p (h d) -> p h d", h=4),
                                                                          axis=AX.X, op=ALU.add), t_a)
                            rstd(rsh[:], ssh[:], 1.0 / 128, t_rd)
                            t13 = t1[:].rearrange("p (h d) -> p h d", h=4)
                            dve.op(lambda: nc.vector.tensor_tensor(
                                out=t13, in0=psrc[:, :].rearrange("p (h d) -> p h d", h=4),
                                in1=rsh[:].unsqueeze(2).to_broadcast([P, 4, P]), op=ALU.mult))
                            dve.op(lambda: nc.vector.tensor_tensor(
                                out=t13, in0=t13, in1=gtile[:].unsqueeze(1).to_broadcast([P, 4, P]), op=ALU.mult), t_tab)
                            t14 = t1[:].rearrange("p (h d two) -> p h d two", h=4, two=2)
                            x0 = t14[:, :, :, 0]
                            x1 = t14[:, :, :, 1]
                            cb = cosb[:, tt, :].unsqueeze(1).to_broadcast([P, 4, 64])
                            sbn = sinb[:, tt, :].unsqueeze(1).to_broadcast([P, 4, 64])
                            m13 = m1[:].rearrange("p (h d) -> p h d", h=4)
                            m23 = m2[:].rearrange("p (h d) -> p h d", h=4)
                            d4 = dst_bf[:].rearrange("p (h d two) -> p h d two", h=4, two=2)
                            dve.op(lambda: nc.vector.tensor_tensor(out=m13, in0=x0, in1=cb, op=ALU.mult))
                            dve.op(lambda: nc.vector.tensor_tensor(out=m23, in0=x1, in1=sbn, op=ALU.mult))
                            dve.op(lambda: nc.vector.tensor_tensor(out=d4[:, :, :, 0], in0=m13, in1=m23, op=ALU.subtract),
                                   dst_free)
                            dve.op(lambda: nc.vector.tensor_tensor(out=m13, in0=x0, in1=sbn, op=ALU.mult))
                            dve.op(lambda: nc.vector.tensor_tensor(out=m23, in0=x1, in1=cb, op=ALU.mult))
                            return dve.op(lambda: nc.vector.tensor_tensor(out=d4[:, :, :, 1], in0=m13, in1=m23, op=ALU.add))

                        for bi, cb_ in enumerate(order):
                            b = 0
                            t_w = dma(sp, wb[b][:], w_in_v[:, :, cb_ * 512:(cb_ + 1) * 512], wb_sl[b],
                                      wb_free[b], cast_w_in.tok())
                            t_last = None
                            if cb_ == 0:
                                t_ev = None
                                for g in range(4):
                                    for tb in range(2):
                                        pb = ppk[0] % 3
                                        ppk[0] += 1
                                        pe.after(t_w, t_hT, pp_free[pb])
                                        for kc in range(KC):
                                            mm = nc.tensor.matmul(pp[pb][:, :], lhsT=wb[b][:, kc, g * P:(g + 1) * P],
                                                                  rhs=hT[:, kc, tb * 512:(tb + 1) * 512],
                                                                  start=(kc == 0), stop=(kc == KC - 1))
                                        t_mm = pe.done(mm)
                                        t_ev = act.op(lambda: nc.scalar.copy(out=fT[:, g, tb * 512:(tb + 1) * 512],
                                                                             in_=pp[pb][:, :]), t_mm)
                                        pp_free[pb] = t_ev
                                t_last = t_mm
                                pz_free = None
                                for ttl in range(8):
                                    tile = th * 8 + ttl
                                    pe.after(t_ev, t_cs, pz_free)
                                    for g in range(4):
                                        mm = nc.tensor.matmul(pz[0][:, g * 256:(g + 1) * 256],
                                                              lhsT=fT[:, g, ttl * P:(ttl + 1) * P], rhs=cst[:, :],
                                                              start=True, stop=True)
                                    t_mm = pe.done(mm)
                                    t_z = dve.op(lambda: nc.vector.tensor_copy(
                                        out=zs[:, tile, :, :].rearrange("p g c -> p (g c)"), in_=pz[0][:, :]), t_mm)
                                    pz_free = t_z
                            else:
                                for ttl in range(8):
                                    tile = th * 8 + ttl
                                    pb = ppk[0] % 3
                                    ppk[0] += 1
                                    pe.after(t_w, t_hT, pp_free[pb])
                                    for kc in range(KC):
                                        mm = nc.tensor.matmul(pp[pb][:, :], lhsT=hT[:, kc, ttl * P:(ttl + 1) * P],
                                                              rhs=wb[b][:, kc, :], start=(kc == 0), stop=(kc == KC - 1))
                                    t_mm = pe.done(mm)
                                    t_last = t_mm
                                    if cb_ == 5:
                                        t_ev = act.op(lambda: nc.scalar.copy(
                                            out=vaug[:, tile, :, 0:128],
                                            in_=pp[pb][:, :].rearrange("p (h d) -> p h d", h=4)), t_mm, t_v1)
                                        pp_free[pb] = t_ev
                                        continue
                                    qb = qk[0] % 2
                                    qk[0] += 1
                                    t_nr = normrope(pp[pb], t_mm, kgb if cb_ == 4 else qgb, tile, qn[qb], qn_free[qb])
                                    pp_free[pb] = t_nr
                                    pe.after(t_nr, ptq_free[qb])
                                    for h in range(4):
                                        mm = nc.tensor.transpose(out=ptq[qb][:, h * P:(h + 1) * P],
                                                                 in_=qn[qb][:, h * P:(h + 1) * P], identity=ident_b[:])
                                    t_tr = pe.done(mm)
                                    qn_free[qb] = t_tr
                                    if cb_ == 4:
                                        t_ev = act.op(lambda: nc.scalar.copy(
                                            out=kT[:, :, tile * P:(tile + 1) * P],
                                            in_=ptq[qb][:, :].rearrange("p (h t) -> p h t", h=4)), t_tr)
                                    else:
                                        t_ev = None
                                        for hh in range(4):
                                            h = 4 * (cb_ - 1) + hh
                                            t_ev = act.op(lambda: nc.scalar.copy(
                                                out=qT[:, h // 3, ttl, (h % 3) * P:(h % 3 + 1) * P],
                                                in_=ptq[qb][:, hh * P:(hh + 1) * P]), t_tr)
                                    ptq_free[qb] = t_ev
                            wb_free[b] = t_last
                    barrier()
                t_proj = []

                with ExitStack() as ph:
                    ct = sb(ph, "ct", [P, KC, 512], BF16)
                    st = sb(ph, "st", [P, KC, 512], BF16)
                    tab_sl = slot("tab")
                    yts = sb(ph, "yts", [P, 4, OWN], BF16)
                    wff = sb(ph, "wff", [P, 4, P], F32)
                    wfb = sb(ph, "wfb", [P, 4, P], BF16)
                    gmb = sb(ph, "gmb", [P, 512], F32)
                    junk = sb(ph, "junk4", [P, 512], F32)
                    fss = sb(ph, "fss", [P, 8], F32)
                    frs = sb(ph, "frs", [P, 8], F32)
                    dve.op(lambda: nc.vector.memset(fss[:], 0.0))
                    mtok = [sb(ph, f"mtok{i}", [P, 512], BF16) for i in range(2)]
                    py = [ps(ph, f"py{i}", [P, 512]) for i in range(2)]
                    pf = [ps(ph, f"pf{i}", [P, 512]) for i in range(2)]
                    ptm = [ps(ph, f"ptm{i}", [P, 512], BF16) for i in range(2)]
                    misc = slot("m")
                    t_wf = dma(sp, wff[:], wf[:, :, :], misc)
                    t_wf = dma(sp, gmb[:], gmerge_b[:, 0:512], misc)
                    t_wfb = dve.op(lambda: nc.vector.tensor_copy(out=wfb[:], in_=wff[:]), t_wf)
                    ctv = ctab.rearrange("(tc p) s -> p tc s", p=P)
                    stv = stab.rearrange("(tc p) s -> p tc s", p=P)
                    tab_free = None
                    py_free = [None, None]
                    k = 0
                    for sbk in range(2):
                        t_t = dma(sp, ct[:], ctv[:, :, sbk * 512:(sbk + 1) * 512], tab_sl, tab_free, cast_tab.tok())
                        t_t = dma(sp, st[:], stv[:, :, sbk * 512:(sbk + 1) * 512], tab_sl)
                        for g in range(4):
                            b = k % 2
                            k += 1
                            pe.after(t_t, py_free[b])
                            for tc in range(KC):
                                nc.tensor.matmul(py[b][:, :], lhsT=zs[:, tc, g, 0:128], rhs=ct[:, tc, :],
                                                 start=(tc == 0), stop=False)
                                mm = nc.tensor.matmul(py[b][:, :], lhsT=zs[:, tc, g, 128:256], rhs=st[:, tc, :],
                                                      start=False, stop=(tc == KC - 1))
                            t_mm = pe.done(mm)
                            t_ev = act.op(lambda: nc.scalar.copy(out=yts[:, g, sbk * 512:(sbk + 1) * 512], in_=py[b][:, :]),
                                          t_mm)
                            py_free[b] = t_ev
                        tab_free = t_mm
                    t_y = t_ev
                    pf_free = [None, None]
                    ptm_free = [None, None]
                    mt_free = [None, None]
                    for tt in range(8):
                        b = tt % 2
                        pe.after(t_y, t_wfb, pf_free[b])
                        for g in range(4):
                            mm = nc.tensor.matmul(pf[b][:, g * P:(g + 1) * P], lhsT=yts[:, g, tt * P:(tt + 1) * P],
                                                  rhs=wfb[:, g, :], start=True, stop=True)
                        t_mm = pe.done(mm)
                        t_sq = act.op(lambda: nc.scalar.activation(out=junk[:], in_=pf[b][:, :], func=AF.Square,
                                                                   accum_out=fss[:, tt:tt + 1]), t_mm)
                        rstd(frs[:, tt:tt + 1], fss[:, tt:tt + 1], 1.0 / 512, t_sq)
                        t_m = dve.op(lambda: nc.vector.scalar_tensor_tensor(
                            out=mtok[b][:], in0=pf[b][:, :], scalar=frs[:, tt:tt + 1], in1=gmb[:],
                            op0=ALU.mult, op1=ALU.mult), mt_free[b])
                        pf_free[b] = t_m
                        pe.after(t_m, ptm_free[b], t_idb)
                        for g in range(4):
                            mm = nc.tensor.transpose(out=ptm[b][:, g * P:(g + 1) * P], in_=mtok[b][:, g * P:(g + 1) * P],
                                                     identity=ident_b[:])
                        t_tr = pe.done(mm)
                        mt_free[b] = t_tr
                        t_ev = act.op(lambda: nc.scalar.copy(
                            out=mergedT[:, 0:4, tt * P:(tt + 1) * P],
                            in_=ptm[b][:, :].rearrange("p (g t) -> p g t", g=4)), t_tr)
                        ptm_free[b] = t_ev
                    t_four = []
                barrier()


            with ExitStack() as ph:
                pt = [sb(ph, f"pt{i}", [P, 16, 384], BF16) for i in range(2)]
                ao = sb(ph, "ao", [P, 1536], F32)
                aob = sb(ph, "aob", [P, 1536], BF16)
                gab = sb(ph, "gab", [P, 1536], F32)
                junk = sb(ph, "junk5", [P, 1536], F32)
                rec = sb(ph, "rec", [P, 4], F32)
                ass = sb(ph, "ass", [P, 8], F32)
                ars = sb(ph, "ars", [P, 8], F32)
                dve.op(lambda: nc.vector.memset(ass[:], 0.0))
                pss = [ps(ph, f"pss{i}", [P, 512]) for i in range(4)]
                po = [ps(ph, f"po{i}", [P, 512]) for i in range(2)]
                pta = ps(ph, "pta", [P, 1536], BF16)
                t_ga = dma(sp, gab[:], gmerge_b[:, 512:2048], slot("m"))
                pss_free = [None] * 4
                pt_free = [None, None]
                po_free = [None, None]
                pta_free = None
                aob_free = None
                ao_free = None
                sk = 0
                u = 0
                scale = float(128 ** -0.5)
                stt = dict(pta_free=None, aob_free=None, ao_free=None, t_aolast=None)

                def emit_qk(qt, g, pb):
                    nonlocal sk
                    t_exp = None
                    for kt in range(16):
                        sbn_ = sk % 4
                        sk += 1
                        pe.after(pss_free[sbn_])
                        mm = nc.tensor.matmul(pss[sbn_][:, 0:384], lhsT=kT[:, g, kt * P:(kt + 1) * P],
                                              rhs=qT[:, g, qt, :], start=True, stop=True)
                        t_mm = pe.done(mm)
                        t_exp = act.op(lambda: nc.scalar.activation(out=pt[pb][:, kt, :], in_=pss[sbn_][:, 0:384],
                                                                    func=AF.Exp, scale=scale),
                                       t_mm, pt_free[pb] if kt == 0 else None, ser=(kt == 0))
                        pss_free[sbn_] = t_exp
                    return t_exp

                def emit_pv(qt, g, pb, t_exp):
                    pe.after(t_exp, po_free[pb])
                    for j in range(3):
                        for kt in range(16):
                            mm = nc.tensor.matmul(po[pb][:, j * 132:j * 132 + 129], lhsT=pt[pb][:, kt, j * P:(j + 1) * P],
                                                  rhs=vaug[:, kt, g, 0:129], start=(kt == 0), stop=(kt == 15))
                    t_pv = pe.done(mm)
                    pt_free[pb] = t_pv
                    po3 = po[pb][:, 0:396].rearrange("p (j c) -> p j c", j=3)
                    dve.op(lambda: nc.vector.reciprocal(out=rec[:, 0:3], in_=po3[:, :, 128]), t_pv)
                    t_ao = dve.op(lambda: nc.vector.tensor_tensor(
                        out=ao[:, g * 384:(g + 1) * 384].rearrange("p (j c) -> p j c", j=3),
                        in0=po3[:, :, 0:128], in1=rec[:, 0:3].unsqueeze(2).to_broadcast([P, 3, P]),
                        op=ALU.mult), stt['ao_free'] if g == 0 else None)
                    po_free[pb] = t_ao
                    if g < 3:
                        return
                    t_sq = act.op(lambda: nc.scalar.activation(out=junk[:], in_=ao[:], func=AF.Square,
                                                               accum_out=ass[:, qt:qt + 1]), t_ao)
                    rstd(ars[:, qt:qt + 1], ass[:, qt:qt + 1], 1.0 / 1536, t_sq)
                    t_ab = dve.op(lambda: nc.vector.scalar_tensor_tensor(
                        out=aob[:], in0=ao[:], scalar=ars[:, qt:qt + 1], in1=gab[:], op0=ALU.mult, op1=ALU.mult),
                        stt['aob_free'], t_ga)
                    stt['ao_free'] = t_ab
                    pe.after(t_ab, stt['pta_free'])
                    for c in range(12):
                        mm = nc.tensor.transpose(out=pta[:, c * P:(c + 1) * P], in_=aob[:, c * P:(c + 1) * P],
                                                 identity=ident_b[:])
                    t_tr = pe.done(mm)
                    stt['aob_free'] = t_tr
                    t_ev = act.op(lambda: nc.scalar.copy(
                        out=mergedT[:, 4:16, qt * P:(qt + 1) * P],
                        in_=pta[:, :].rearrange("p (c t) -> p c t", c=12)), t_tr)
                    stt['pta_free'] = t_ev

                prev = None
                for qt in range(8):
                    for g in range(4):
                        pb = u % 2
                        u += 1
                        t_exp = emit_qk(qt, g, pb)
                        if prev is not None:
                            emit_pv(*prev)
                        prev = (qt, g, pb, t_exp)
                emit_pv(*prev)
                t_attn = []
                barrier()
            kvq.close()

            with ExitStack() as ph:
                wo = sb(ph, "wo", [P, KC, D], BF16)
                wo_sl = slot("wo")
                gm_row = sb(ph, "gm_row6", [P, D], F32)
                t_gm = dma(sp, gm_row[:], gm_scr[:, :], slot("m"))
                wr = sb(ph, "wr", [P, KC, 32], F32)
                brb = sb(ph, "brb", [P, 32], F32)
                xo = [sb(ph, f"xo{i}", [P, D], F32) for i in range(2)]
                xo_sl = [slot("xo") for _ in range(2)]
                x1 = [sb(ph, f"x1_{i}", [P, D], F32) for i in range(2)]
                x1_sl = [slot("x1s") for _ in range(2)]
                xs2 = sb(ph, "xs2", [P, D], F32)
                junk = sb(ph, "junk6", [P, D], F32)
                h2f = sb(ph, "h2f", [P, KC, P], F32)
                h2b = [sb(ph, f"h2b{i}", [P, KC, P], BF16) for i in range(2)]
                h2_sl = [slot("h2s") for _ in range(2)]
                sm = sb(ph, "sm", [P, 16], F32)
                lg = sb(ph, "lg", [P, 32], F32)
                ex = sb(ph, "ex", [P, 32], F32)
                msk = sb(ph, "msk", [P, 32], F32)
                top8 = sb(ph, "top8", [P, 8], F32)
                gt = [sb(ph, f"gt{i}", [P, 32], F32) for i in range(2)]
                gt_sl = [slot("gts") for _ in range(2)]
                pmb = [ps(ph, f"pm{i}", [P, D]) for i in range(2)]
                w_out_v = w_out_bf.rearrange("(kc p) n -> p kc n", p=P)
                t_wo = None
                for h in range(4):
                    t_wo = dma(sp, wo[:, :, h * 512:(h + 1) * 512], w_out_v[:, :, h * 512:(h + 1) * 512], wo_sl,
                               cast_w_out.tok(), *t_attn)
                misc = slot("m")
                t_wr = dma(sp, wr[:], w_router[:, :, :], misc)
                t_wr = dma(sp, brb[:], b_router_b[:, :], misc)
                xo_free = [None, None]
                x1_free = [[], []]
                h2b_free = [None, None]
                gt_free = [None, None]
                pm_free = [None, None]
                t_op = {}

                def outproj(tt):
                    pm = pmb[tt % 2]
                    pe.after(t_wo, pm_free[tt % 2])
                    for db in range(4):
                        for fc in range(KC):
                            mm = nc.tensor.matmul(pm[:, db * 512:(db + 1) * 512], lhsT=mergedT[:, fc, tt * P:(tt + 1) * P],
                                                  rhs=wo[:, fc, db * 512:(db + 1) * 512],
                                                  start=(fc == 0), stop=(fc == KC - 1))
                    t_op[tt] = pe.done(mm)

                outproj(0)
                for tt in range(8):
                    b = tt % 2
                    pm = pmb[b]
                    t_xo = dma(sp, xo[b][:], xp[tt * P:(tt + 1) * P, :], xo_sl[b], xo_free[b])
                    if tt + 1 < 8:
                        outproj(tt + 1)
                    t_mm = t_op[tt]
                    t_z = dve.op(lambda: nc.vector.memset(sm[:], 0.0))
                    for db in range(4):
                        t_sq = act.op(lambda: nc.scalar.activation(out=junk[:, db * 512:(db + 1) * 512],
                                                                   in_=pm[:, db * 512:(db + 1) * 512], func=AF.Square,
                                                                   accum_out=sm[:, db:db + 1]), t_mm, t_z, ser=(db == 0))
                    t_rd = dve.op(lambda: nc.vector.tensor_reduce(out=sm[:, 4:5], in_=sm[:, 0:4], axis=AX.X, op=ALU.add), t_sq)
                    rstd(sm[:, 5:6], sm[:, 4:5], 1.0 / D, t_rd)
                    t_a = None
                    for db in range(4):
                        t_a = dve.op(lambda: nc.vector.scalar_tensor_tensor(
                            out=x1[b][:, db * 512:(db + 1) * 512], in0=pm[:, db * 512:(db + 1) * 512],
                            scalar=sm[:, 5:6], in1=gm_row[:, db * 512:(db + 1) * 512], op0=ALU.mult, op1=ALU.mult),
                            t_gm, *(x1_free[b] if db == 0 else []), ser=(db == 0))
                    t_x1 = dve.op(lambda: nc.vector.tensor_tensor(out=x1[b][:], in0=x1[b][:], in1=xo[b][:], op=ALU.add),
                                  t_xo)
                    xo_free[b] = t_x1
                    t_st = dma(sp, x1_scr[tt * P:(tt + 1) * P, :], x1[b][:], x1_sl[b], t_x1)
                    t_sq = act.op(lambda: nc.scalar.activation(out=junk[:], in_=x1[b][:], func=AF.Square,
                                                               accum_out=sm[:, 8:9]), t_x1)
                    t_r = rstd(sm[:, 9:10], sm[:, 8:9], 1.0 / D, t_sq)
                    t_xs = act.op(lambda: nc.scalar.activation(out=xs2[:], in_=x1[b][:], func=AF.Identity, scale=sm[:, 9:10]),
                                  t_r)
                    x1_free[b] = [t_xs, t_st]
                    pe.after(t_xs, t_a)
                    for dc in range(KC):
                        mm = nc.tensor.transpose(out=pm[:, dc * P:(dc + 1) * P], in_=xs2[:, dc * P:(dc + 1) * P],
                                                 identity=ident_f[:])
                    t_tr = pe.done(mm)
                    t_h2 = None
                    for dc in range(KC):
                        t_h2 = act.op(lambda: nc.scalar.activation(
                            out=h2f[:, dc, :], in_=pm[:, dc * P:(dc + 1) * P], func=AF.Identity,
                            scale=a_f[:, dc:dc + 1], bias=modcol[:, 32 + dc:33 + dc]), t_tr, ser=(dc == 0))
                    t_hb = dve.op(lambda: nc.vector.tensor_copy(out=h2b[b][:], in_=h2f[:]), t_h2, h2b_free[b])
                    h2b_free[b] = dma(sp, ht_send.rearrange("(dc p) t -> p dc t", p=P)[:, :, tt * P:(tt + 1) * P],
                                      h2b[b][:], h2_sl[b], t_hb)
                    pe.after(t_h2, t_wr)
                    for dc in range(KC):
                        mm = nc.tensor.matmul(pm[:, 0:32], lhsT=h2f[:, dc, :], rhs=wr[:, dc, :],
                                              start=(dc == 0), stop=(dc == KC - 1))
                    t_lg = pe.done(mm)
                    t_l = dve.op(lambda: nc.vector.tensor_tensor(out=lg[:], in0=pm[:, 0:32], in1=brb[:], op=ALU.add), t_lg)
                    pm_free[b] = t_l
                    dve.op(lambda: nc.vector.max(out=top8[:], in_=lg[:]))
                    dve.op(lambda: nc.vector.tensor_scalar(out=msk[:], in0=lg[:], scalar1=top8[:, 3:4], scalar2=None,
                                                           op0=ALU.is_ge))
                    t_n = dve.op(lambda: nc.vector.tensor_scalar(out=top8[:, 4:5], in0=top8[:, 0:1], scalar1=-1.0,
                                                                 scalar2=None, op0=ALU.mult))
                    t_e = act.op(lambda: nc.scalar.activation(out=ex[:], in_=lg[:], func=AF.Exp, bias=top8[:, 4:5],
                                                              scale=1.0), t_n)
                    dve.op(lambda: nc.vector.tensor_tensor(out=ex[:], in0=ex[:], in1=msk[:], op=ALU.mult), t_e)
                    dve.op(lambda: nc.vector.tensor_reduce(out=top8[:, 5:6], in_=ex[:], axis=AX.X, op=ALU.add))
                    dve.op(lambda: nc.vector.reciprocal(out=top8[:, 6:7], in_=top8[:, 5:6]))
                    t_g = dve.op(lambda: nc.vector.tensor_scalar(out=gt[b][:], in0=ex[:], scalar1=top8[:, 6:7],
                                                                 scalar2=None, op0=ALU.mult), gt_free[b])
                    gt_free[b] = dma(sp, g_send[tt * P:(tt + 1) * P, :], gt[b][:], gt_sl[b], t_g)
                t_send = [x1_sl[0].tok(), x1_sl[1].tok(), h2_sl[0].tok(), h2_sl[1].tok(), gt_sl[0].tok(), gt_sl[1].tok()]
                t_ph7 = [('e', pe, pe.n), ('e', act, act.n), ('e', dve, dve.n)]
                barrier(*t_send)

        NE = 32
        NU = 16

        def barrier_all(*extra):
            toks = [('e', pe, pe.n), ('e', act, act.n), ('e', dve, dve.n), ('e', pool, pool.n)] + list(extra)
            for en in (pe, act, dve, pool, sp):
                en.after(*toks)

        barrier_all(*t_send)
        with ExitStack() as ph0:
            yacc = sb(ph0, "yacc", [P, 8, D], F32)
            ph = ph0.enter_context(ExitStack())
            hb = sb(ph, "hb", [P, KC, OWN], BF16)
            gb = sb(ph, "gb", [P, 8, 32], F32)
            NS = 2
            NW = 3
            sg_f = [sb(ph, f"sgf{i}", [P, KC, P], F32) for i in range(NS)]
            su_f = [sb(ph, f"suf{i}", [P, KC, P], F32) for i in range(NS)]
            sd_f = [sb(ph, f"sdf{i}", [P, D], F32) for i in range(NS)]
            s_sl = [slot("stg") for _ in range(NS)]
            NGU, NWD, NAV = 2, 5, 4
            wgs = [sb(ph, f"wgs{i}", [P, KC, P], BF16) for i in range(NGU)]
            wus = [sb(ph, f"wus{i}", [P, KC, P], BF16) for i in range(NGU)]
            wds = [sb(ph, f"wds{i}", [P, D], BF16) for i in range(NWD)]
            bdt = sb(ph, "bdt", [P, 512], F32)
            bd_sl = slot("bd")
            bgc = sb(ph, "bgc", [P, NE, KC], F32)
            buc = sb(ph, "buc", [P, NE, KC], F32)
            av = [sb(ph, f"av{i}", [P, OWN], BF16) for i in range(NAV)]
            gcl = [sb(ph, f"gcl{i}", [P, 512], F32) for i in range(2)]
            sg1 = sb(ph, "sg1", [P, 512], F32)
            sg = [sg1, sg1]
            ucl = [sb(ph, f"ucl{i}", [P, 512], F32) for i in range(2)]
            pg = [ps(ph, f"pg{i}", [P, 512]) for i in range(2)]
            pu = [ps(ph, f"pu{i}", [P, 512]) for i in range(2)]
            pd = [ps(ph, f"pd{i}", [P, 512]) for i in range(4)]
            misc = slot("m")
            t_b = dma(sp, bgc[:], bg_col[:, :, :], misc)
            t_b = dma(sp, buc[:], bu_col[:, :, :], misc)
            t_b = dma(sp, hb[:], ht_send.rearrange("(dc p) t -> p dc t", p=P), misc)
            t_b = dma(sp, gb[:], g_send.rearrange("(tt p) e -> p tt e", p=P), misc)
            t_b = dve.op(lambda: nc.vector.tensor_scalar(out=buc[:], in0=buc[:], scalar1=1.0, scalar2=None, op0=ALU.add),
                         t_b)
            wg_v = wg.rearrange("e (kc p) n -> e p kc n", p=P)
            wu_v = wu.rearrange("e (kc p) n -> e p kc n", p=P)
            wd_v = wd.rearrange("e (fc p) n -> e p fc n", p=P)
            st = dict(s_free=[[None, None, None] for _ in range(NS)], gu_free=[None] * NGU, wd_free=[None] * NWD,
                      sg_free=None,
                      pg_free=[None, None], pu_free=[None, None], pd_free=[None] * 4,
                      av_free=[None] * NAV, bd_free=None, dk=0, tmp_free=[None, None])
            units = [(e, un) for e in range(NE) for un in range(NU)]
            PRE = 2

            def issue_w(k):
                e, un = units[k]
                s_ = k % NS
                sp.after(*st['s_free'][s_])
                d1 = s_sl[s_].add(nc.sync.dma_start(out=sg_f[s_][:], in_=wg_v[e][:, :, un * P:(un + 1) * P]))
                d2 = s_sl[s_].add(nc.sync.dma_start(out=su_f[s_][:], in_=wu_v[e][:, :, un * P:(un + 1) * P]))
                d3 = s_sl[s_].add(nc.sync.dma_start(out=sd_f[s_][:], in_=wd_v[e][:, un, :]))
                c1 = act.op(lambda: nc.scalar.copy(out=wgs[k % NGU][:], in_=sg_f[s_][:]), d3, st['gu_free'][k % NGU])
                c2 = act.op(lambda: nc.scalar.copy(out=wus[k % NGU][:], in_=su_f[s_][:]), ser=False)
                c3 = act.op(lambda: nc.scalar.copy(out=wds[k % NWD][:], in_=sd_f[s_][:]), st['wd_free'][k % NWD], ser=False)
                st['s_free'][s_] = [c1, c2, c3]
                return (c1, c2, c3)

            w_tok = {}
            for i in range(PRE):
                w_tok[i] = issue_w(i)

            def pair_tasks(ia, ib):
                for tt in range(8):
                    for db in range(4):
                        yield (ia, ib, tt, db, tt == 7 and db == 3)

            def do_task(task):
                (ia, ib, tt, db, last) = task
                pb = st['dk'] % 4
                st['dk'] += 1
                pe.after(ia['t_avs'][tt // 4], ib['t_avs'][tt // 4], ia['c3'], ib['c3'], st['pd_free'][pb])
                nc.tensor.matmul(pd[pb][:, :], lhsT=av[ia['av']][:, tt * P:(tt + 1) * P],
                                 rhs=wds[ia['wd']][:, db * 512:(db + 1) * 512], start=True, stop=False)
                mm = nc.tensor.matmul(pd[pb][:, :], lhsT=av[ib['av']][:, tt * P:(tt + 1) * P],
                                      rhs=wds[ib['wd']][:, db * 512:(db + 1) * 512], start=False, stop=True)
                t_mm = pe.done(mm)
                e = ia['e']
                t_evl = dve.op(lambda: nc.vector.scalar_tensor_tensor(
                    out=yacc[:, tt, db * 512:(db + 1) * 512], in0=pd[pb][:, :],
                    scalar=gb[:, tt, e:e + 1],
                    in1=yacc[:, tt, db * 512:(db + 1) * 512], op0=ALU.mult, op1=ALU.add), t_mm, ser=False)
                st['pd_free'][pb] = t_evl
                if last:
                    for info in (ia, ib):
                        st['av_free'][info['av']] = t_mm
                        st['wd_free'][info['wd']] = t_mm
                return t_evl

            def run_tasks(gen, n):
                t = None
                for _ in range(n):
                    task = next(gen, None)
                    if task is None:
                        break
                    t = do_task(task)
                return t

            gen = iter(())
            prev = None
            t_dn = None
            for ui, (e, un) in enumerate(units):
                for db_ in (range(4) if un == 0 else []):
                    t_bd = dma(sp, bdt[:], bd_b[e][:, db_ * 512:(db_ + 1) * 512], bd_sl, st['bd_free'])
                    t_i = None
                    for tt in range(8):
                        if e == 0:
                            t_i = dve.op(lambda: nc.vector.tensor_scalar(
                                out=yacc[:, tt, db_ * 512:(db_ + 1) * 512], in0=bdt[:], scalar1=gb[:, tt, e:e + 1],
                                scalar2=None, op0=ALU.mult), t_bd, t_b)
                        else:
                            t_i = dve.op(lambda: nc.vector.scalar_tensor_tensor(
                                out=yacc[:, tt, db_ * 512:(db_ + 1) * 512], in0=bdt[:], scalar=gb[:, tt, e:e + 1],
                                in1=yacc[:, tt, db_ * 512:(db_ + 1) * 512], op0=ALU.mult, op1=ALU.add), t_bd)
                    st['bd_free'] = t_i
                gu = ui % NGU
                ab = ui % NAV
                (c1, c2, c3) = w_tok.pop(ui)
                t_avs = []
                cnt = 0
                t_u = None
                for tb in range(2):
                    pe.after(c1, t_b, st['pg_free'][tb])
                    for kc in range(KC):
                        mm = nc.tensor.matmul(pg[tb][:, :], lhsT=wgs[gu][:, kc, :], rhs=hb[:, kc, tb * 512:(tb + 1) * 512],
                                              start=(kc == 0), stop=(kc == KC - 1))
                        if kc == KC - 1:
                            t_g = pe.done(mm)
                        cnt += 1
                        if cnt % 4 == 0:
                            run_tasks(gen, 1)
                    t_gc = dve.op(lambda: nc.vector.tensor_scalar(out=gcl[tb][:], in0=pg[tb][:, :],
                                                                  scalar1=bgc[:, e, un:un + 1], scalar2=LIMIT,
                                                                  op0=ALU.add, op1=ALU.min), t_g, t_b, st['tmp_free'][tb])
                    st['pg_free'][tb] = t_gc
                    t_s = act.op(lambda: nc.scalar.activation(out=sg[tb][:], in_=gcl[tb][:], func=AF.Sigmoid, scale=ALPHA),
                                 t_gc, st['sg_free'])
                    pe.after(c2, st['pu_free'][tb])
                    for kc in range(KC):
                        mm = nc.tensor.matmul(pu[tb][:, :], lhsT=wus[gu][:, kc, :], rhs=hb[:, kc, tb * 512:(tb + 1) * 512],
                                              start=(kc == 0), stop=(kc == KC - 1))
                        if kc == KC - 1:
                            t_u = pe.done(mm)
                        cnt += 1
                        if cnt % 4 == 0:
                            run_tasks(gen, 1)
                    t_ur = act.op(lambda: nc.scalar.activation(out=ucl[tb][:], in_=pu[tb][:, :], func=AF.Identity,
                                                               bias=buc[:, e, un:un + 1], scale=1.0), t_u, st['tmp_free'][tb])
                    st['pu_free'][tb] = t_ur
                    t_gs = pool.op(lambda: nc.gpsimd.tensor_tensor(out=gcl[tb][:], in0=gcl[tb][:], in1=sg[tb][:],
                                                                   op=ALU.mult), t_s)
                    st['sg_free'] = t_gs
                    pool.op(lambda: nc.gpsimd.tensor_scalar(out=ucl[tb][:], in0=ucl[tb][:], scalar1=LIMIT + 1.0,
                                                            scalar2=-LIMIT + 1.0, op0=ALU.min, op1=ALU.max), t_ur)
                    t_av = pool.op(lambda: nc.gpsimd.tensor_tensor(
                        out=av[ab][:, tb * 512:(tb + 1) * 512], in0=ucl[tb][:], in1=gcl[tb][:],
                        op=ALU.mult), st['av_free'][ab] if tb == 0 else None)
                    st['tmp_free'][tb] = t_av
                    t_avs.append(t_av)
                st['gu_free'][gu] = t_u
                info = dict(e=e, av=ab, wd=ui % NWD, t_avs=t_avs, c3=c3)
                if ui % 2 == 1:
                    run_tasks(gen, 64)
                    gen = pair_tasks(prev, info)
                else:
                    prev = info
                if ui + PRE < len(units):
                    w_tok[ui + PRE] = issue_w(ui + PRE)
            t_dn = run_tasks(gen, 64)
            barrier_all()
            ph.close()
            ph = ph0

            xr = [sb(ph, f"xr{i}", [P, D], F32) for i in range(2)]
            ld_sl = [slot("fl") for _ in range(2)]
            st_sl = [slot("fs") for _ in range(2)]
            junk = sb(ph, "junk9", [P, D], F32)
            fs = sb(ph, "fs", [P, 16], F32)
            gf_row = sb(ph, "gf_row10", [P, D], F32)
            t_gf = dma(sp, gf_row[:], gf_scr[:, :], slot("m"))
            dve.op(lambda: nc.vector.memset(fs[:], 0.0))
            st_free = [None, None]
            for tt in range(8):
                b = tt % 2
                t_ld = dma(sp, xr[b][:], x1_scr[tt * P:(tt + 1) * P, :], ld_sl[b], st_free[b])
                t_sq = act.op(lambda: nc.scalar.activation(out=junk[:], in_=yacc[:, tt, :], func=AF.Square,
                                                           accum_out=fs[:, tt:tt + 1]), t_dn)
                rstd(fs[:, 8 + tt:9 + tt], fs[:, tt:tt + 1], 1.0 / D, t_sq)
                dve.op(lambda: nc.vector.scalar_tensor_tensor(out=yacc[:, tt, :], in0=yacc[:, tt, :],
                                                              scalar=fs[:, 8 + tt:9 + tt],
                                                              in1=gf_row[:], op0=ALU.mult, op1=ALU.mult), t_gf)
                t_o = dve.op(lambda: nc.vector.tensor_tensor(out=xr[b][:], in0=yacc[:, tt, :], in1=xr[b][:], op=ALU.add),
                             t_ld)
                st_free[b] = dma(sp, out[tt * P:(tt + 1) * P, :], xr[b][:], st_sl[b], t_o)
            sp.after(st_sl[0].tok(), st_sl[1].tok())
    return nc


_CACHE = {}


def _bf(a):
    return np.ascontiguousarray(a.astype(ml_dtypes.bfloat16))


def _f(a):
    return np.ascontiguousarray(a, dtype=np.float32)


def _col(v):
    return _f(np.asarray(v).reshape(KC, P).T)


def _rep(v):
    return _f(np.broadcast_to(np.asarray(v)[None, :], (P, v.shape[-1])))


def kernel(x, c, w_ada, b_ada, g_pre_mix, w_in, w_fourier, q_norm_g, k_norm_g, g_fourier_out, g_attn_out, w_out,
           g_post_mix, g_pre_ffn, w_router, b_router, w_gate, b_gate, w_up, b_up, w_down, b_down, g_post_ffn):
    arrs = [np.asarray(a) for a in (x, c, w_ada, b_ada, g_pre_mix, w_in, w_fourier, q_norm_g, k_norm_g,
                                    g_fourier_out, g_attn_out, w_out, g_post_mix, g_pre_ffn, w_router, b_router,
                                    w_gate, b_gate, w_up, b_up, w_down, b_down, g_post_ffn)]
    (x, c, w_ada, b_ada, g_pre_mix, w_in, w_fourier, q_norm_g, k_norm_g, g_fourier_out, g_attn_out, w_out,
     g_post_mix, g_pre_ffn, w_router, b_router, w_gate, b_gate, w_up, b_up, w_down, b_down, g_post_ffn) = arrs
    if 'nc' not in _CACHE:
        _CACHE['nc'] = build()
    nc = _CACHE['nc']
    inv_freq = (1.0 / (10000.0 ** (np.arange(0, 64, 2, dtype=np.float32) / 64.0))).astype(np.float32)
    cc64 = np.arange(128, dtype=np.int64)
    ang_c = 2.0 * np.pi * ((cc64[:, None] * cc64[None, :]) % 128).astype(np.float64) / 128.0
    cs_tab = _f(np.concatenate([np.cos(ang_c) / 512.0, -np.sin(ang_c) / 512.0], axis=1))
    ident = np.eye(P, dtype=np.float32)
    shared = dict(
        w_ada=_f(w_ada[0]), b_ada=_f(b_ada[0][None, :]), gpm_col=_col(g_pre_mix[0]), gpf_col=_col(g_pre_ffn[0]),
        w_in=_f(w_in[0]), w_out=_f(w_out[0]), wf=_f(np.transpose(w_fourier[0], (1, 0, 2))),
        qg_b=_rep(q_norm_g[0]), kg_b=_rep(k_norm_g[0]),
        gmerge_b=_rep(np.concatenate([g_fourier_out[0], g_attn_out[0]])),
        gpostm_b=_rep(g_post_mix[0]), gpostf_b=_rep(g_post_ffn[0]),
        w_router=_f(np.transpose(w_router[0].reshape(KC, P, 32), (1, 0, 2))), b_router_b=_rep(b_router[0]),
        cs_tab=cs_tab, ident_in=ident,
        wg=_f(w_gate[0]), wu=_f(w_up[0]), wd=_f(w_down[0]),
        bg_col=_f(np.transpose(b_gate[0].reshape(32, KC, P), (2, 0, 1))),
        bu_col=_f(np.transpose(b_up[0].reshape(32, KC, P), (2, 0, 1))),
        bd_b=_f(np.broadcast_to(b_down[0][:, None, :], (32, P, D))),
    )
    in_maps = []
    for core in range(NCORE):
        b, half = core // 2, core % 2
        own = np.arange(half * OWN, (half + 1) * OWN)
        oth = np.arange((1 - half) * OWN, (2 - half) * OWN)
        perm = np.concatenate([own, oth])
        pos = perm.astype(np.float32)
        row_idx = np.floor(pos / 64.0).astype(np.float32)
        col_idx = (pos - row_idx * 64.0).astype(np.float32)
        ang = np.concatenate([row_idx[:, None] * inv_freq[None, :], col_idx[:, None] * inv_freq[None, :]],
                             axis=1).astype(np.float32)
        cos_t = np.cos(ang).astype(np.float32).reshape(16, P, 64).transpose(1, 0, 2)
        sin_t = np.sin(ang).astype(np.float32).reshape(16, P, 64).transpose(1, 0, 2)
        prod = (perm.astype(np.int64)[:, None] * own.astype(np.int64)[None, :]) % S
        ang_s = 2.0 * np.pi * prod.astype(np.float64) / S
        m = dict(shared)
        m.update(
            xp=_f(x[b][perm]), c_col=_col(c[b]),
            cos_t=_f(cos_t), sin_t=_f(sin_t), ctab=_f(np.cos(ang_s)), stab=_f(np.sin(ang_s)),
        )
        in_maps.append(m)
    res = run_bass_kernel_spmd(nc, in_maps, core_ids=list(range(NCORE)))
    outp = np.empty((4, S, D), np.float32)
    for core in range(NCORE):
        b, half = core // 2, core % 2
        outp[b, half * OWN:(half + 1) * OWN] = res.results[core]["out"]
    return outp
```

```python
import numpy as np
import ml_dtypes
from contextlib import ExitStack
import concourse.bass as bass
import concourse.mybir as mybir
from concourse.bass_utils import run_bass_kernel_spmd

F32 = mybir.dt.float32
BF16 = mybir.dt.bfloat16
ALU = mybir.AluOpType
AF = mybir.ActivationFunctionType
AX = mybir.AxisListType

P = 128
D = 2048
KC = 16
S = 2048
OWN = 1024
NCORE = 8
NE_OWN = 4
EPS = 1e-6
LIMIT = 7.0
ALPHA = 1.702


class Eng:
    def __init__(self, nc, eng, sem, serial):
        self.nc, self.eng, self.sem, self.n = nc, eng, sem, 0
        self.waited = {}
        self.serial = serial

    def after(self, *toks):
        for t in toks:
            if t is None:
                continue
            if t[0] == 'd':
                _, sem, val = t
                key = ('d', id(sem))
                if self.waited.get(key, 0) < val:
                    self.eng.wait_ge(sem, val)
                    self.waited[key] = val
            else:
                _, prod, n = t
                if prod is self and not self.serial:
                    continue
                key = ('e', id(prod))
                if self.waited.get(key, 0) < n:
                    self.eng.wait_ge(prod.sem, n)
                    self.waited[key] = n

    def done(self, instr):
        self.n += 1
        instr.then_inc(self.sem, 1)
        return ('e', self, self.n)

    def op(self, fn, *deps, ser=True):
        self.after(*deps)
        if self.serial and ser and self.n > 0:
            self.after(('e', self, self.n))
        return self.done(fn())


class Slot:
    def __init__(self, sem):
        self.sem, self.cnt = sem, 0

    def add(self, instr):
        self.cnt += 16
        instr.then_inc(self.sem, 16)
        return ('d', self.sem, self.cnt)

    def tok(self):
        return ('d', self.sem, self.cnt)


def build(stage=99):
    nc = bass.Bass("TRN2", target_bir_lowering=False)

    def din(name, shape, dt=F32):
        return nc.dram_tensor(name, list(shape), dt, kind="ExternalInput").ap()

    def dscr(name, shape, dt):
        return nc.dram_tensor(name, list(shape), dt, kind="Internal").ap()

    xp = din("xp", [S, D])
    c_col = din("c_col", [P, KC])
    w_ada = din("w_ada", [D, 6 * D])
    b_ada = din("b_ada", [1, 6 * D])
    gpm_col = din("gpm_col", [P, KC])
    gpf_col = din("gpf_col", [P, KC])
    w_in = din("w_in", [D, 3072])
    w_out = din("w_out", [D, D])
    wf = din("wf", [P, 4, P])
    qg_b = din("qg_b", [P, P])
    kg_b = din("kg_b", [P, P])
    gmerge_b = din("gmerge_b", [P, D])
    gpostm_b = din("gpostm_b", [P, D])
    gpostf_b = din("gpostf_b", [P, D])
    w_router = din("w_router", [P, KC, 32])
    b_router_b = din("b_router_b", [P, 32])
    wg = din("wg", [32, D, D])
    wu = din("wu", [32, D, D])
    wd = din("wd", [32, D, D])
    bg_col = din("bg_col", [P, 32, KC])
    bu_col = din("bu_col", [P, 32, KC])
    bd_b = din("bd_b", [32, P, D])
    cos_t = din("cos_t", [P, 16, 64])
    sin_t = din("sin_t", [P, 16, 64])
    ctab_f = din("ctab", [S, OWN])
    stab_f = din("stab", [S, OWN])
    cs_tab_f = din("cs_tab", [P, 256])
    ident_in = din("ident_in", [P, P])
    out = nc.dram_tensor("out", [OWN, D], F32, kind="ExternalOutput").ap()

    w_in_bf = dscr("w_in_bf", [D, 3072], BF16)
    ctab = dscr("ctab_bf", [S, OWN], BF16)
    stab = dscr("stab_bf", [S, OWN], BF16)
    cs_tab = dscr("cs_tab_bf", [P, 256], BF16)
    w_out_bf = dscr("w_out_bf", [D, D], BF16)
    x1_scr = dscr("x1_scr", [OWN, D], F32)
    ht_send = dscr("ht_send", [D, OWN], BF16)
    g_send = dscr("g_send", [OWN, 32], F32)
    gm_scr = dscr("gm_scr", [P, D], F32)
    gf_scr = dscr("gf_scr", [P, D], F32)

    with ExitStack() as top:
        nsem = [0]

        def newsem(name):
            nsem[0] += 1
            return top.enter_context(nc.semaphore(f"{name}_{nsem[0]}"))

        def slot(name="dq"):
            return Slot(newsem(name))

        pe = Eng(nc, nc.tensor, newsem("pe"), False)
        act = Eng(nc, nc.scalar, newsem("act"), True)
        dve = Eng(nc, nc.vector, newsem("dve"), True)
        pool = Eng(nc, nc.gpsimd, newsem("pool"), True)
        sp = Eng(nc, nc.sync, newsem("sp"), False)

        def sb(ctx, name, shape, dt):
            nsem[0] += 1
            return ctx.enter_context(nc.sbuf_tensor(f"{name}_{nsem[0]}", list(shape), dt))

        def ps(ctx, name, shape, dt=F32):
            nsem[0] += 1
            return ctx.enter_context(nc.psum_tensor(f"{name}_{nsem[0]}", list(shape), dt))

        def dma(q, out_ap, in_ap, sl, *deps):
            q.after(*deps)
            return sl.add(q.eng.dma_start(out=out_ap, in_=in_ap))

        def rstd(out_ap, in_ap, inv_n, *deps):
            t_a = act.op(lambda: nc.scalar.activation(out=out_ap, in_=in_ap, func=AF.Sqrt, scale=inv_n,
                                                      bias=eps_t[:, 0:1]), *deps)
            return dve.op(lambda: nc.vector.reciprocal(out=out_ap, in_=out_ap), t_a)

        def barrier(*extra):
            toks = [('e', pe, pe.n), ('e', act, act.n), ('e', dve, dve.n)] + list(extra)
            for en in (pe, act, dve, sp):
                en.after(*toks)

        cast_w_in = slot("cwi")
        cast_w_out = slot("cwo")
        for h in range(2):
            cast_w_in.add(nc.gpsimd.dma_start(out=w_in_bf[:, h * 1536:(h + 1) * 1536],
                                              in_=w_in[:, h * 1536:(h + 1) * 1536]))
        cast_tab = slot("ctb")
        cast_tab.add(nc.gpsimd.dma_start(out=cs_tab[:, :], in_=cs_tab_f[:, :]))
        cast_tab.add(nc.gpsimd.dma_start(out=ctab[:, :], in_=ctab_f[:, :]))
        cast_tab.add(nc.gpsimd.dma_start(out=stab[:, :], in_=stab_f[:, :]))
        cast_w_out.add(nc.gpsimd.dma_start(out=w_out_bf[:, :], in_=w_out[:, :]))

        ident_f = sb(top, "ident_f", [P, P], F32)
        ident_b = sb(top, "ident_b", [P, P], BF16)
        ones_f = sb(top, "ones_f", [P, P], F32)
        modcol = sb(top, "modcol", [P, 64], F32)
        a_m = sb(top, "a_m", [P, KC], F32)
        a_f = sb(top, "a_f", [P, KC], F32)
        t_id = dma(sp, ident_f[:], ident_in[:, :], slot("m"))
        t_idb = dve.op(lambda: nc.vector.tensor_copy(out=ident_b[:], in_=ident_f[:]), t_id)
        t_ones = dve.op(lambda: nc.vector.memset(ones_f[:], 1.0))
        eps_t = sb(top, "eps_t", [P, 1], F32)
        dve.op(lambda: nc.vector.memset(eps_t[:], EPS))

        with ExitStack() as ph:
            NWA = 3
            wa = [sb(ph, f"wa{i}", [P, KC, 512], F32) for i in range(NWA)]
            wa_sl = [slot("wa") for _ in range(NWA)]
            modrow = sb(ph, "modrow", [1, 6 * D], F32)
            cc = sb(ph, "cc", [P, KC], F32)
            sc = sb(ph, "sc", [P, KC], F32)
            gpm = sb(ph, "gpm", [P, KC], F32)
            gpf = sb(ph, "gpf", [P, KC], F32)
            gpb = sb(ph, "gpb", [P, D], F32)
            gm_row = sb(ph, "gm_row", [P, D], F32)
            gf_row = sb(ph, "gf_row", [P, D], F32)
            ps_row = [ps(ph, f"ps_row{i}", [P, 512]) for i in range(2)]
            ps_col = ps(ph, "ps_col", [P, 64])
            ps_bc = [ps(ph, f"ps_bc{i}", [P, 512]) for i in range(2)]
            misc = slot("m")
            t_c = dma(sp, cc[:], c_col[:, :], misc)
            t_c = dma(sp, modrow[:], b_ada[:, :], misc)
            t_c = dma(sp, gpm[:], gpm_col[:, :], misc)
            t_c = dma(sp, gpf[:], gpf_col[:, :], misc)
            t_sg = act.op(lambda: nc.scalar.activation(out=sc[:], in_=cc[:], func=AF.Sigmoid), t_c)
            t_sc = dve.op(lambda: nc.vector.tensor_tensor(out=sc[:], in0=sc[:], in1=cc[:], op=ALU.mult), t_sg)
            w_ada_v = w_ada.rearrange("(kc p) n -> p kc n", p=P)
            wa_free = [None] * NWA
            psr_free = [None, None]
            t_row = None
            t_lds = {}
            for nb in range(NWA - 1):
                t_lds[nb] = dma(sp, wa[nb % NWA][:], w_ada_v[:, :, nb * 512:(nb + 1) * 512], wa_sl[nb % NWA])
            for nb in range(24):
                b = nb % 2
                wb_ = nb % NWA
                nx = nb + NWA - 1
                if nx < 24:
                    t_lds[nx] = dma(sp, wa[nx % NWA][:], w_ada_v[:, :, nx * 512:(nx + 1) * 512], wa_sl[nx % NWA],
                                    wa_free[nx % NWA])
                pe.after(t_lds.pop(nb), t_sc, psr_free[b])
                for kc in range(KC):
                    mm = nc.tensor.matmul(ps_row[b][0:1, :], lhsT=sc[:, kc:kc + 1], rhs=wa[wb_][:, kc, :],
                                          start=(kc == 0), stop=(kc == KC - 1))
                t_mm = pe.done(mm)
                wa_free[wb_] = t_mm
                t_row = dve.op(lambda: nc.vector.tensor_tensor(
                    out=modrow[0:1, nb * 512:(nb + 1) * 512], in0=ps_row[b][0:1, :],
                    in1=modrow[0:1, nb * 512:(nb + 1) * 512], op=ALU.add), t_mm, t_c)
                psr_free[b] = t_row
            pe.after(t_row, t_ones)
            for v, j in enumerate((0, 1, 3, 4)):
                for c in range(KC):
                    mm = nc.tensor.matmul(ps_col[:, v * 16 + c:v * 16 + c + 1],
                                          lhsT=modrow[0:1, j * D + c * P:j * D + (c + 1) * P],
                                          rhs=ones_f[0:1, 0:1], start=True, stop=True)
            t_mm = pe.done(mm)
            t_mc = dve.op(lambda: nc.vector.tensor_copy(out=modcol[:], in_=ps_col[:]), t_mm)
            dve.op(lambda: nc.vector.scalar_tensor_tensor(out=a_m[:], in0=modcol[:, 16:32], scalar=1.0,
                                                          in1=gpm[:], op0=ALU.add, op1=ALU.mult))
            t_am = dve.op(lambda: nc.vector.scalar_tensor_tensor(out=a_f[:], in0=modcol[:, 48:64], scalar=1.0,
                                                                 in1=gpf[:], op0=ALU.add, op1=ALU.mult))
            bc_free = [None, None]
            k = 0
            for (j, dst, gsrc) in ((2, gm_row, gpostm_b), (5, gf_row, gpostf_b)):
                t_g = dma(sp, gpb[:], gsrc[:, :], slot("m"), t_am if k == 0 else t_bc)
                for nb in range(4):
                    b = k % 2
                    pe.after(bc_free[b])
                    mm = nc.tensor.matmul(ps_bc[b][:, :], lhsT=ones_f[0:1, :],
                                          rhs=modrow[0:1, j * D + nb * 512:j * D + (nb + 1) * 512],
                                          start=True, stop=True)
                    t_mm = pe.done(mm)
                    t_bc = dve.op(lambda: nc.vector.tensor_tensor(out=dst[:, nb * 512:(nb + 1) * 512],
                                                                  in0=ps_bc[b][:, :],
                                                                  in1=gpb[:, nb * 512:(nb + 1) * 512],
                                                                  op=ALU.mult), t_mm, t_g)
                    bc_free[b] = t_bc
                    k += 1
            g_sl = slot("gs")
            dma(sp, gm_scr[:, :], gm_row[:], g_sl, t_bc)
            t_gs = dma(sp, gf_scr[:, :], gf_row[:], g_sl)
            barrier(t_gs)

        with ExitStack() as mx:
            mergedT = sb(mx, "mergedT", [P, KC, OWN], BF16)
            cosb = sb(mx, "cosb", [P, 16, 64], F32)
            sinb = sb(mx, "sinb", [P, 16, 64], F32)
            qgb = sb(mx, "qgb", [P, P], F32)
            kgb = sb(mx, "kgb", [P, P], F32)
            kvq = mx.enter_context(ExitStack())
            kT = sb(kvq, "kT", [P, 4, S], BF16)
            vaug = sb(kvq, "vaug", [P, 16, 4, 132], BF16)
            qT = sb(kvq, "qT", [P, 4, 8, 384], BF16)
            misc = slot("m")
            t_tab = dma(sp, cosb[:], cos_t[:, :, :], misc)
            t_tab = dma(sp, sinb[:], sin_t[:, :, :], misc)
            t_tab = dma(sp, qgb[:], qg_b[:, :], misc)
            t_tab = dma(sp, kgb[:], kg_b[:, :], misc)
            t_v1 = dve.op(lambda: nc.vector.memset(vaug[:, :, :, 128:129], 1.0))

            with ExitStack() as zsc:
                zs = sb(zsc, "zs", [P, 16, 4, 256], BF16)
                cst = sb(zsc, "cst", [P, 256], BF16)
                t_cs = dma(sp, cst[:], cs_tab[:, :], slot("m"), cast_tab.tok())
                w_in_v = w_in_bf.rearrange("(kc p) n -> p kc n", p=P)
                for th in range(2):
                  with ExitStack() as pj:
                    hT = sb(pj, "hT", [P, KC, OWN], BF16)
                    with ExitStack() as ph:
                        xt = [sb(ph, f"xt{i}", [P, D], F32) for i in range(2)]
                        xt_sl = [slot("xt") for _ in range(2)]
                        xs = [sb(ph, f"xs{i}", [P, D], BF16) for i in range(2)]
                        junk = sb(ph, "junk", [P, D], F32)
                        ss = sb(ph, "ss", [P, 8], F32)
                        rs = sb(ph, "rs", [P, 8], F32)
                        dve.op(lambda: nc.vector.memset(ss[:], 0.0))
                        pst = [ps(ph, f"pst{i}", [P, 512], BF16) for i in range(4)]
                        xt_free = [None, None]
                        xs_free = [None, None]
                        pst_free = [None] * 4
                        kk = 0
                        t_ev = None
                        for i in range(8):
                            b = i % 2
                            tile = th * 8 + i
                            t_ld = dma(sp, xt[b][:], xp[tile * P:(tile + 1) * P, :], xt_sl[b], xt_free[b])
                            t_sq = act.op(lambda: nc.scalar.activation(out=junk[:], in_=xt[b][:], func=AF.Square,
                                                                       accum_out=ss[:, i:i + 1]), t_ld)
                            t_rs = rstd(rs[:, i:i + 1], ss[:, i:i + 1], 1.0 / D, t_sq)
                            t_xs = act.op(lambda: nc.scalar.activation(out=xs[b][:], in_=xt[b][:],
                                                                       func=AF.Identity, scale=rs[:, i:i + 1]),
                                          t_rs, xs_free[b])
                            xt_free[b] = t_xs
                            t_tr = None
                            for q4 in range(4):
                                pb = kk % 4
                                kk += 1
                                pe.after(t_xs, t_idb, pst_free[pb])
                                for c4 in range(4):
                                    dc = q4 * 4 + c4
                                    mm = nc.tensor.transpose(out=pst[pb][:, c4 * P:(c4 + 1) * P],
                                                             in_=xs[b][:, dc * P:(dc + 1) * P], identity=ident_b[:])
                                t_tr = pe.done(mm)
                                for c4 in range(4):
                                    dc = q4 * 4 + c4
                                    t_ev = act.op(lambda: nc.scalar.activation(
                                        out=hT[:, dc, i * P:(i + 1) * P], in_=pst[pb][:, c4 * P:(c4 + 1) * P],
                                        func=AF.Identity, scale=a_m[:, dc:dc + 1], bias=modcol[:, dc:dc + 1]), t_tr,
                                        ser=(c4 == 0))
                                pst_free[pb] = t_ev
                            xs_free[b] = t_tr
                        t_hT = t_ev
                    barrier()

                    with ExitStack() as ph:
                        wb = [sb(ph, f"wb{i}", [P, KC, 512], BF16) for i in range(1)]
                        wb_sl = [slot("wb") for _ in range(1)]
                        fT = sb(ph, "fT", [P, 4, OWN], BF16)
                        sq = sb(ph, "sq", [P, 512], F32)
                        t1 = sb(ph, "t1", [P, 512], F32)
                        m1 = sb(ph, "m1", [P, 256], F32)
                        m2 = sb(ph, "m2", [P, 256], F32)
                        qn = [sb(ph, f"qn{i}", [P, 512], BF16) for i in range(2)]
                        ssh = sb(ph, "ssh", [P, 4], F32)
                        rsh = sb(ph, "rsh", [P, 4], F32)
                        pp = [ps(ph, f"pp{i}", [P, 512]) for i in range(3)]
                        pz = [ps(ph, f"pz{i}", [P, 1024]) for i in range(1)]
                        ptq = [ps(ph, f"ptq{i}", [P, 512], BF16) for i in range(2)]
                        wb_free = [None, None]
                        pp_free = [None] * 3
                        ptq_free = [None, None]
                        qn_free = [None, None]
                        ppk = [0]
                        qk = [0]
                        order = [0, 4, 5] + ([1, 2, 3] if th == 0 else [])

                        def normrope(psrc, t_mm, gtile, tt, dst_bf, dst_free):
                            t_a = act.op(lambda: nc.scalar.activation(out=sq[:], in_=psrc[:, :], func=AF.Square), t_mm)
                            t_rd = dve.op(lambda: nc.vector.tensor_reduce(out=ssh[:], in_=sq[:].rearrange("p (h d) -> p h d", h=4),
                                                                          axis=AX.X, op=ALU.add), t_a)
                            rstd(rsh[:], ssh[:], 1.0 / 128, t_rd)
                            t13 = t1[:].rearrange("p (h d) -> p h d", h=4)
                            dve.op(lambda: nc.vector.tensor_tensor(
                                out=t13, in0=psrc[:, :].rearrange("p (h d) -> p h d", h=4),
                                in1=rsh[:].unsqueeze(2).to_broadcast([P, 4, P]), op=ALU.mult))
                            dve.op(lambda: nc.vector.tensor_tensor(
                                out=t13, in0=t13, in1=gtile[:].unsqueeze(1).to_broadcast([P, 4, P]), op=ALU.mult), t_tab)
                            t14 = t1[:].rearrange("p (h d two) -> p h d two", h=4, two=2)
                            x0 = t14[:, :, :, 0]
                            x1 = t14[:, :, :, 1]
                            cb = cosb[:, tt, :].unsqueeze(1).to_broadcast([P, 4, 64])
                            sbn = sinb[:, tt, :].unsqueeze(1).to_broadcast([P, 4, 64])
                            m13 = m1[:].rearrange("p (h d) -> p h d", h=4)
                            m23 = m2[:].rearrange("p (h d) -> p h d", h=4)
                            d4 = dst_bf[:].rearrange("p (h d two) -> p h d two", h=4, two=2)
                            dve.op(lambda: nc.vector.tensor_tensor(out=m13, in0=x0, in1=cb, op=ALU.mult))
                            dve.op(lambda: nc.vector.tensor_tensor(out=m23, in0=x1, in1=sbn, op=ALU.mult))
                            dve.op(lambda: nc.vector.tensor_tensor(out=d4[:, :, :, 0], in0=m13, in1=m23, op=ALU.subtract),
                                   dst_free)
                            dve.op(lambda: nc.vector.tensor_tensor(out=m13, in0=x0, in1=sbn, op=ALU.mult))
                            dve.op(lambda: nc.vector.tensor_tensor(out=m23, in0=x1, in1=cb, op=ALU.mult))
                            return dve.op(lambda: nc.vector.tensor_tensor(out=d4[:, :, :, 1], in0=m13, in1=m23, op=ALU.add))

                        for bi, cb_ in enumerate(order):
                            b = 0
                            t_w = dma(sp, wb[b][:], w_in_v[:, :, cb_ * 512:(cb_ + 1) * 512], wb_sl[b],
                                      wb_free[b], cast_w_in.tok())
                            t_last = None
                            if cb_ == 0:
                                t_ev = None
                                for g in range(4):
                                    for tb in range(2):
                                        pb = ppk[0] % 3
                                        ppk[0] += 1
                                        pe.after(t_w, t_hT, pp_free[pb])
                                        for kc in range(KC):
                                            mm = nc.tensor.matmul(pp[pb][:, :], lhsT=wb[b][:, kc, g * P:(g + 1) * P],
                                                                  rhs=hT[:, kc, tb * 512:(tb + 1) * 512],
                                                                  start=(kc == 0), stop=(kc == KC - 1))
                                        t_mm = pe.done(mm)
                                        t_ev = act.op(lambda: nc.scalar.copy(out=fT[:, g, tb * 512:(tb + 1) * 512],
                                                                             in_=pp[pb][:, :]), t_mm)
                                        pp_free[pb] = t_ev
                                t_last = t_mm
                                pz_free = None
                                for ttl in range(8):
                                    tile = th * 8 + ttl
                                    pe.after(t_ev, t_cs, pz_free)
                                    for g in range(4):
                                        mm = nc.tensor.matmul(pz[0][:, g * 256:(g + 1) * 256],
                                                              lhsT=fT[:, g, ttl * P:(ttl + 1) * P], rhs=cst[:, :],
                                                              start=True, stop=True)
                                    t_mm = pe.done(mm)
                                    t_z = dve.op(lambda: nc.vector.tensor_copy(
                                        out=zs[:, tile, :, :].rearrange("p g c -> p (g c)"), in_=pz[0][:, :]), t_mm)
                                    pz_free = t_z
                            else:
                                for ttl in range(8):
                                    tile = th * 8 + ttl
                                    pb = ppk[0] % 3
                                    ppk[0] += 1
                                    pe.after(t_w, t_hT, pp_free[pb])
                                    for kc in range(KC):
                                        mm = nc.tensor.matmul(pp[pb][:, :], lhsT=hT[:, kc, ttl * P:(ttl + 1) * P],
                                                              rhs=wb[b][:, kc, :], start=(kc == 0), stop=(kc == KC - 1))
                                    t_mm = pe.done(mm)
                                    t_last = t_mm
                                    if cb_ == 5:
                                        t_ev = act.op(lambda: nc.scalar.copy(
                                            out=vaug[:, tile, :, 0:128],
                                            in_=pp[pb][:, :].rearrange("p (h d) -> p h d", h=4)), t_mm, t_v1)
                                        pp_free[pb] = t_ev
                                        continue
                                    qb = qk[0] % 2
                                    qk[0] += 1
                                    t_nr = normrope(pp[pb], t_mm, kgb if cb_ == 4 else qgb, tile, qn[qb], qn_free[qb])
                                    pp_free[pb] = t_nr
                                    pe.after(t_nr, ptq_free[qb])
                                    for h in range(4):
                                        mm = nc.tensor.transpose(out=ptq[qb][:, h * P:(h + 1) * P],
                                                                 in_=qn[qb][:, h * P:(h + 1) * P], identity=ident_b[:])
                                    t_tr = pe.done(mm)
                                    qn_free[qb] = t_tr
                                    if cb_ == 4:
                                        t_ev = act.op(lambda: nc.scalar.copy(
                                            out=kT[:, :, tile * P:(tile + 1) * P],
                                            in_=ptq[qb][:, :].rearrange("p (h t) -> p h t", h=4)), t_tr)
                                    else:
                                        t_ev = None
                                        for hh in range(4):
                                            h = 4 * (cb_ - 1) + hh
                                            t_ev = act.op(lambda: nc.scalar.copy(
                                                out=qT[:, h // 3, ttl, (h % 3) * P:(h % 3 + 1) * P],
                                                in_=ptq[qb][:, hh * P:(hh + 1) * P]), t_tr)
                                    ptq_free[qb] = t_ev
                            wb_free[b] = t_last
                    barrier()
                t_proj = []

                with ExitStack() as ph:
                    ct = sb(ph, "ct", [P, KC, 512], BF16)
                    st = sb(ph, "st", [P, KC, 512], BF16)
                    tab_sl = slot("tab")
                    yts = sb(ph, "yts", [P, 4, OWN], BF16)
                    wff = sb(ph, "wff", [P, 4, P], F32)
                    wfb = sb(ph, "wfb", [P, 4, P], BF16)
                    gmb = sb(ph, "gmb", [P, 512], F32)
                    junk = sb(ph, "junk4", [P, 512], F32)
                    fss = sb(ph, "fss", [P, 8], F32)
                    frs = sb(ph, "frs", [P, 8], F32)
                    dve.op(lambda: nc.vector.memset(fss[:], 0.0))
                    mtok = [sb(ph, f"mtok{i}", [P, 512], BF16) for i in range(2)]
                    py = [ps(ph, f"py{i}", [P, 512]) for i in range(2)]
                    pf = [ps(ph, f"pf{i}", [P, 512]) for i in range(2)]
                    ptm = [ps(ph, f"ptm{i}", [P, 512], BF16) for i in range(2)]
                    misc = slot("m")
                    t_wf = dma(sp, wff[:], wf[:, :, :], misc)
                    t_wf = dma(sp, gmb[:], gmerge_b[:, 0:512], misc)
                    t_wfb = dve.op(lambda: nc.vector.tensor_copy(out=wfb[:], in_=wff[:]), t_wf)
                    ctv = ctab.rearrange("(tc p) s -> p tc s", p=P)
                    stv = stab.rearrange("(tc p) s -> p tc s", p=P)
                    tab_free = None
                    py_free = [None, None]
                    k = 0
                    for sbk in range(2):
                        t_t = dma(sp, ct[:], ctv[:, :, sbk * 512:(sbk + 1) * 512], tab_sl, tab_free, cast_tab.tok())
                        t_t = dma(sp, st[:], stv[:, :, sbk * 512:(sbk + 1) * 512], tab_sl)
                        for g in range(4):
                            b = k % 2
                            k += 1
                            pe.after(t_t, py_free[b])
                            for tc in range(KC):
                                nc.tensor.matmul(py[b][:, :], lhsT=zs[:, tc, g, 0:128], rhs=ct[:, tc, :],
                                                 start=(tc == 0), stop=False)
                                mm = nc.tensor.matmul(py[b][:, :], lhsT=zs[:, tc, g, 128:256], rhs=st[:, tc, :],
                                                      start=False, stop=(tc == KC - 1))
                            t_mm = pe.done(mm)
                            t_ev = act.op(lambda: nc.scalar.copy(out=yts[:, g, sbk * 512:(sbk + 1) * 512], in_=py[b][:, :]),
                                          t_mm)
                            py_free[b] = t_ev
                        tab_free = t_mm
                    t_y = t_ev
                    pf_free = [None, None]
                    ptm_free = [None, None]
                    mt_free = [None, None]
                    for tt in range(8):
                        b = tt % 2
                        pe.after(t_y, t_wfb, pf_free[b])
                        for g in range(4):
                            mm = nc.tensor.matmul(pf[b][:, g * P:(g + 1) * P], lhsT=yts[:, g, tt * P:(tt + 1) * P],
                                                  rhs=wfb[:, g, :], start=True, stop=True)
                        t_mm = pe.done(mm)
                        t_sq = act.op(lambda: nc.scalar.activation(out=junk[:], in_=pf[b][:, :], func=AF.Square,
                                                                   accum_out=fss[:, tt:tt + 1]), t_mm)
                        rstd(frs[:, tt:tt + 1], fss[:, tt:tt + 1], 1.0 / 512, t_sq)
                        t_m = dve.op(lambda: nc.vector.scalar_tensor_tensor(
                            out=mtok[b][:], in0=pf[b][:, :], scalar=frs[:, tt:tt + 1], in1=gmb[:],
                            op0=ALU.mult, op1=ALU.mult), mt_free[b])
                        pf_free[b] = t_m
                        pe.after(t_m, ptm_free[b], t_idb)
                        for g in range(4):
                            mm = nc.tensor.transpose(out=ptm[b][:, g * P:(g + 1) * P], in_=mtok[b][:, g * P:(g + 1) * P],
                                                     identity=ident_b[:])
                        t_tr = pe.done(mm)
                        mt_free[b] = t_tr
                        t_ev = act.op(lambda: nc.scalar.copy(
                            out=mergedT[:, 0:4, tt * P:(tt + 1) * P],
                            in_=ptm[b][:, :].rearrange("p (g t) -> p g t", g=4)), t_tr)
                        ptm_free[b] = t_ev
                    t_four = []
                barrier()


            with ExitStack() as ph:
                pt = [sb(ph, f"pt{i}", [P, 16, 384], BF16) for i in range(2)]
                ao = sb(ph, "ao", [P, 1536], F32)
                aob = sb(ph, "aob", [P, 1536], BF16)
                gab = sb(ph, "gab", [P, 1536], F32)
                junk = sb(ph, "junk5", [P, 1536], F32)
                rec = sb(ph, "rec", [P, 4], F32)
                ass = sb(ph, "ass", [P, 8], F32)
                ars = sb(ph, "ars", [P, 8], F32)
                dve.op(lambda: nc.vector.memset(ass[:], 0.0))
                pss = [ps(ph, f"pss{i}", [P, 512]) for i in range(4)]
                po = [ps(ph, f"po{i}", [P, 512]) for i in range(2)]
                pta = ps(ph, "pta", [P, 1536], BF16)
                t_ga = dma(sp, gab[:], gmerge_b[:, 512:2048], slot("m"))
                pss_free = [None] * 4
                pt_free = [None, None]
                po_free = [None, None]
                pta_free = None
                aob_free = None
                ao_free = None
                sk = 0
                u = 0
                scale = float(128 ** -0.5)
                stt = dict(pta_free=None, aob_free=None, ao_free=None, t_aolast=None)

                def emit_qk(qt, g, pb):
                    nonlocal sk
                    t_exp = None
                    for kt in range(16):
                        sbn_ = sk % 4
                        sk += 1
                        pe.after(pss_free[sbn_])
                        mm = nc.tensor.matmul(pss[sbn_][:, 0:384], lhsT=kT[:, g, kt * P:(kt + 1) * P],
                                              rhs=qT[:, g, qt, :], start=True, stop=True)
                        t_mm = pe.done(mm)
                        t_exp = act.op(lambda: nc.scalar.activation(out=pt[pb][:, kt, :], in_=pss[sbn_][:, 0:384],
                                                                    func=AF.Exp, scale=scale),
                                       t_mm, pt_free[pb] if kt == 0 else None, ser=(kt == 0))
                        pss_free[sbn_] = t_exp
                    return t_exp

                def emit_pv(qt, g, pb, t_exp):
                    pe.after(t_exp, po_free[pb])
                    for j in range(3):
                        for kt in range(16):
                            mm = nc.tensor.matmul(po[pb][:, j * 132:j * 132 + 129], lhsT=pt[pb][:, kt, j * P:(j + 1) * P],
                                                  rhs=vaug[:, kt, g, 0:129], start=(kt == 0), stop=(kt == 15))
                    t_pv = pe.done(mm)
                    pt_free[pb] = t_pv
                    po3 = po[pb][:, 0:396].rearrange("p (j c) -> p j c", j=3)
                    dve.op(lambda: nc.vector.reciprocal(out=rec[:, 0:3], in_=po3[:, :, 128]), t_pv)
                    t_ao = dve.op(lambda: nc.vector.tensor_tensor(
                        out=ao[:, g * 384:(g + 1) * 384].rearrange("p (j c) -> p j c", j=3),
                        in0=po3[:, :, 0:128], in1=rec[:, 0:3].unsqueeze(2).to_broadcast([P, 3, P]),
                        op=ALU.mult), stt['ao_free'] if g == 0 else None)
                    po_free[pb] = t_ao
                    if g < 3:
                        return
                    t_sq = act.op(lambda: nc.scalar.activation(out=junk[:], in_=ao[:], func=AF.Square,
                                                               accum_out=ass[:, qt:qt + 1]), t_ao)
                    rstd(ars[:, qt:qt + 1], ass[:, qt:qt + 1], 1.0 / 1536, t_sq)
                    t_ab = dve.op(lambda: nc.vector.scalar_tensor_tensor(
                        out=aob[:], in0=ao[:], scalar=ars[:, qt:qt + 1], in1=gab[:], op0=ALU.mult, op1=ALU.mult),
                        stt['aob_free'], t_ga)
                    stt['ao_free'] = t_ab
                    pe.after(t_ab, stt['pta_free'])
                    for c in range(12):
                        mm = nc.tensor.transpose(out=pta[:, c * P:(c + 1) * P], in_=aob[:, c * P:(c + 1) * P],
                                                 identity=ident_b[:])
                    t_tr = pe.done(mm)
                    stt['aob_free'] = t_tr
                    t_ev = act.op(lambda: nc.scalar.copy(
                        out=mergedT[:, 4:16, qt * P:(qt + 1) * P],
                        in_=pta[:, :].rearrange("p (c t) -> p c t", c=12)), t_tr)
                    stt['pta_free'] = t_ev

                prev = None
                for qt in range(8):
                    for g in range(4):
                        pb = u % 2
                        u += 1
                        t_exp = emit_qk(qt, g, pb)
                        if prev is not None:
                            emit_pv(*prev)
                        prev = (qt, g, pb, t_exp)
                emit_pv(*prev)
                t_attn = []
                barrier()
            kvq.close()

            with ExitStack() as ph:
                wo = sb(ph, "wo", [P, KC, D], BF16)
                wo_sl = slot("wo")
                gm_row = sb(ph, "gm_row6", [P, D], F32)
                t_gm = dma(sp, gm_row[:], gm_scr[:, :], slot("m"))
                wr = sb(ph, "wr", [P, KC, 32], F32)
                brb = sb(ph, "brb", [P, 32], F32)
                xo = [sb(ph, f"xo{i}", [P, D], F32) for i in range(2)]
                xo_sl = [slot("xo") for _ in range(2)]
                x1 = [sb(ph, f"x1_{i}", [P, D], F32) for i in range(2)]
                x1_sl = [slot("x1s") for _ in range(2)]
                xs2 = sb(ph, "xs2", [P, D], F32)
                junk = sb(ph, "junk6", [P, D], F32)
                h2f = sb(ph, "h2f", [P, KC, P], F32)
                h2b = [sb(ph, f"h2b{i}", [P, KC, P], BF16) for i in range(2)]
                h2_sl = [slot("h2s") for _ in range(2)]
                sm = sb(ph, "sm", [P, 16], F32)
                lg = sb(ph, "lg", [P, 32], F32)
                ex = sb(ph, "ex", [P, 32], F32)
                msk = sb(ph, "msk", [P, 32], F32)
                top8 = sb(ph, "top8", [P, 8], F32)
                gt = [sb(ph, f"gt{i}", [P, 32], F32) for i in range(2)]
                gt_sl = [slot("gts") for _ in range(2)]
                pmb = [ps(ph, f"pm{i}", [P, D]) for i in range(2)]
                w_out_v = w_out_bf.rearrange("(kc p) n -> p kc n", p=P)
                t_wo = None
                for h in range(4):
                    t_wo = dma(sp, wo[:, :, h * 512:(h + 1) * 512], w_out_v[:, :, h * 512:(h + 1) * 512], wo_sl,
                               cast_w_out.tok(), *t_attn)
                misc = slot("m")
                t_wr = dma(sp, wr[:], w_router[:, :, :], misc)
                t_wr = dma(sp, brb[:], b_router_b[:, :], misc)
                xo_free = [None, None]
                x1_free = [[], []]
                h2b_free = [None, None]
                gt_free = [None, None]
                pm_free = [None, None]
                t_op = {}

                def outproj(tt):
                    pm = pmb[tt % 2]
                    pe.after(t_wo, pm_free[tt % 2])
                    for db in range(4):
                        for fc in range(KC):
                            mm = nc.tensor.matmul(pm[:, db * 512:(db + 1) * 512], lhsT=mergedT[:, fc, tt * P:(tt + 1) * P],
                                                  rhs=wo[:, fc, db * 512:(db + 1) * 512],
                                                  start=(fc == 0), stop=(fc == KC - 1))
                    t_op[tt] = pe.done(mm)

                outproj(0)
                for tt in range(8):
                    b = tt % 2
                    pm = pmb[b]
                    t_xo = dma(sp, xo[b][:], xp[tt * P:(tt + 1) * P, :], xo_sl[b], xo_free[b])
                    if tt + 1 < 8:
                        outproj(tt + 1)
                    t_mm = t_op[tt]
                    t_z = dve.op(lambda: nc.vector.memset(sm[:], 0.0))
                    for db in range(4):
                        t_sq = act.op(lambda: nc.scalar.activation(out=junk[:, db * 512:(db + 1) * 512],
                                                                   in_=pm[:, db * 512:(db + 1) * 512], func=AF.Square,
                                                                   accum_out=sm[:, db:db + 1]), t_mm, t_z, ser=(db == 0))
                    t_rd = dve.op(lambda: nc.vector.tensor_reduce(out=sm[:, 4:5], in_=sm[:, 0:4], axis=AX.X, op=ALU.add), t_sq)
                    rstd(sm[:, 5:6], sm[:, 4:5], 1.0 / D, t_rd)
                    t_a = None
                    for db in range(4):
                        t_a = dve.op(lambda: nc.vector.scalar_tensor_tensor(
                            out=x1[b][:, db * 512:(db + 1) * 512], in0=pm[:, db * 512:(db + 1) * 512],
                            scalar=sm[:, 5:6], in1=gm_row[:, db * 512:(db + 1) * 512], op0=ALU.mult, op1=ALU.mult),
                            t_gm, *(x1_free[b] if db == 0 else []), ser=(db == 0))
                    t_x1 = dve.op(lambda: nc.vector.tensor_tensor(out=x1[b][:], in0=x1[b][:], in1=xo[b][:], op=ALU.add),
                                  t_xo)
                    xo_free[b] = t_x1
                    t_st = dma(sp, x1_scr[tt * P:(tt + 1) * P, :], x1[b][:], x1_sl[b], t_x1)
                    t_sq = act.op(lambda: nc.scalar.activation(out=junk[:], in_=x1[b][:], func=AF.Square,
                                                               accum_out=sm[:, 8:9]), t_x1)
                    t_r = rstd(sm[:, 9:10], sm[:, 8:9], 1.0 / D, t_sq)
                    t_xs = act.op(lambda: nc.scalar.activation(out=xs2[:], in_=x1[b][:], func=AF.Identity, scale=sm[:, 9:10]),
                                  t_r)
                    x1_free[b] = [t_xs, t_st]
                    pe.after(t_xs, t_a)
                    for dc in range(KC):
                        mm = nc.tensor.transpose(out=pm[:, dc * P:(dc + 1) * P], in_=xs2[:, dc * P:(dc + 1) * P],
                                                 identity=ident_f[:])
                    t_tr = pe.done(mm)
                    t_h2 = None
                    for dc in range(KC):
                        t_h2 = act.op(lambda: nc.scalar.activation(
                            out=h2f[:, dc, :], in_=pm[:, dc * P:(dc + 1) * P], func=AF.Identity,
                            scale=a_f[:, dc:dc + 1], bias=modcol[:, 32 + dc:33 + dc]), t_tr, ser=(dc == 0))
                    t_hb = dve.op(lambda: nc.vector.tensor_copy(out=h2b[b][:], in_=h2f[:]), t_h2, h2b_free[b])
                    h2b_free[b] = dma(sp, ht_send.rearrange("(dc p) t -> p dc t", p=P)[:, :, tt * P:(tt + 1) * P],
                                      h2b[b][:], h2_sl[b], t_hb)
                    pe.after(t_h2, t_wr)
                    for dc in range(KC):
                        mm = nc.tensor.matmul(pm[:, 0:32], lhsT=h2f[:, dc, :], rhs=wr[:, dc, :],
                                              start=(dc == 0), stop=(dc == KC - 1))
                    t_lg = pe.done(mm)
                    t_l = dve.op(lambda: nc.vector.tensor_tensor(out=lg[:], in0=pm[:, 0:32], in1=brb[:], op=ALU.add), t_lg)
                    pm_free[b] = t_l
                    dve.op(lambda: nc.vector.max(out=top8[:], in_=lg[:]))
                    dve.op(lambda: nc.vector.tensor_scalar(out=msk[:], in0=lg[:], scalar1=top8[:, 3:4], scalar2=None,
                                                           op0=ALU.is_ge))
                    t_n = dve.op(lambda: nc.vector.tensor_scalar(out=top8[:, 4:5], in0=top8[:, 0:1], scalar1=-1.0,
                                                                 scalar2=None, op0=ALU.mult))
                    t_e = act.op(lambda: nc.scalar.activation(out=ex[:], in_=lg[:], func=AF.Exp, bias=top8[:, 4:5],
                                                              scale=1.0), t_n)
                    dve.op(lambda: nc.vector.tensor_tensor(out=ex[:], in0=ex[:], in1=msk[:], op=ALU.mult), t_e)
                    dve.op(lambda: nc.vector.tensor_reduce(out=top8[:, 5:6], in_=ex[:], axis=AX.X, op=ALU.add))
                    dve.op(lambda: nc.vector.reciprocal(out=top8[:, 6:7], in_=top8[:, 5:6]))
                    t_g = dve.op(lambda: nc.vector.tensor_scalar(out=gt[b][:], in0=ex[:], scalar1=top8[:, 6:7],
                                                                 scalar2=None, op0=ALU.mult), gt_free[b])
                    gt_free[b] = dma(sp, g_send[tt * P:(tt + 1) * P, :], gt[b][:], gt_sl[b], t_g)
                t_send = [x1_sl[0].tok(), x1_sl[1].tok(), h2_sl[0].tok(), h2_sl[1].tok(), gt_sl[0].tok(), gt_sl[1].tok()]
                t_ph7 = [('e', pe, pe.n), ('e', act, act.n), ('e', dve, dve.n)]
                barrier(*t_send)

        NE = 32
        NU = 16

        def barrier_all(*extra):
            toks = [('e', pe, pe.n), ('e', act, act.n), ('e', dve, dve.n), ('e', pool, pool.n)] + list(extra)
            for en in (pe, act, dve, pool, sp):
                en.after(*toks)

        barrier_all(*t_send)
        with ExitStack() as ph0:
            yacc = sb(ph0, "yacc", [P, 8, D], F32)
            ph = ph0.enter_context(ExitStack())
            hb = sb(ph, "hb", [P, KC, OWN], BF16)
            gb = sb(ph, "gb", [P, 8, 32], F32)
            NS = 2
            NW = 3
            sg_f = [sb(ph, f"sgf{i}", [P, KC, P], F32) for i in range(NS)]
            su_f = [sb(ph, f"suf{i}", [P, KC, P], F32) for i in range(NS)]
            sd_f = [sb(ph, f"sdf{i}", [P, D], F32) for i in range(NS)]
            s_sl = [slot("stg") for _ in range(NS)]
            NGU, NWD, NAV = 2, 5, 4
            wgs = [sb(ph, f"wgs{i}", [P, KC, P], BF16) for i in range(NGU)]
            wus = [sb(ph, f"wus{i}", [P, KC, P], BF16) for i in range(NGU)]
            wds = [sb(ph, f"wds{i}", [P, D], BF16) for i in range(NWD)]
            bdt = sb(ph, "bdt", [P, 512], F32)
            bd_sl = slot("bd")
            bgc = sb(ph, "bgc", [P, NE, KC], F32)
            buc = sb(ph, "buc", [P, NE, KC], F32)
            av = [sb(ph, f"av{i}", [P, OWN], BF16) for i in range(NAV)]
            gcl = [sb(ph, f"gcl{i}", [P, 512], F32) for i in range(2)]
            sg1 = sb(ph, "sg1", [P, 512], F32)
            sg = [sg1, sg1]
            ucl = [sb(ph, f"ucl{i}", [P, 512], F32) for i in range(2)]
            pg = [ps(ph, f"pg{i}", [P, 512]) for i in range(2)]
            pu = [ps(ph, f"pu{i}", [P, 512]) for i in range(2)]
            pd = [ps(ph, f"pd{i}", [P, 512]) for i in range(4)]
            misc = slot("m")
            t_b = dma(sp, bgc[:], bg_col[:, :, :], misc)
            t_b = dma(sp, buc[:], bu_col[:, :, :], misc)
            t_b = dma(sp, hb[:], ht_send.rearrange("(dc p) t -> p dc t", p=P), misc)
            t_b = dma(sp, gb[:], g_send.rearrange("(tt p) e -> p tt e", p=P), misc)
            t_b = dve.op(lambda: nc.vector.tensor_scalar(out=buc[:], in0=buc[:], scalar1=1.0, scalar2=None, op0=ALU.add),
                         t_b)
            wg_v = wg.rearrange("e (kc p) n -> e p kc n", p=P)
            wu_v = wu.rearrange("e (kc p) n -> e p kc n", p=P)
            wd_v = wd.rearrange("e (fc p) n -> e p fc n", p=P)
            st = dict(s_free=[[None, None, None] for _ in range(NS)], gu_free=[None] * NGU, wd_free=[None] * NWD,
                      sg_free=None,
                      pg_free=[None, None], pu_free=[None, None], pd_free=[None] * 4,
                      av_free=[None] * NAV, bd_free=None, dk=0, tmp_free=[None, None])
            units = [(e, un) for e in range(NE) for un in range(NU)]
            PRE = 2

            def issue_w(k):
                e, un = units[k]
                s_ = k % NS
                sp.after(*st['s_free'][s_])
                d1 = s_sl[s_].add(nc.sync.dma_start(out=sg_f[s_][:], in_=wg_v[e][:, :, un * P:(un + 1) * P]))
                d2 = s_sl[s_].add(nc.sync.dma_start(out=su_f[s_][:], in_=wu_v[e][:, :, un * P:(un + 1) * P]))
                d3 = s_sl[s_].add(nc.sync.dma_start(out=sd_f[s_][:], in_=wd_v[e][:, un, :]))
                c1 = act.op(lambda: nc.scalar.copy(out=wgs[k % NGU][:], in_=sg_f[s_][:]), d3, st['gu_free'][k % NGU])
                c2 = act.op(lambda: nc.scalar.copy(out=wus[k % NGU][:], in_=su_f[s_][:]), ser=False)
                c3 = act.op(lambda: nc.scalar.copy(out=wds[k % NWD][:], in_=sd_f[s_][:]), st['wd_free'][k % NWD], ser=False)
                st['s_free'][s_] = [c1, c2, c3]
                return (c1, c2, c3)

            w_tok = {}
            for i in range(PRE):
                w_tok[i] = issue_w(i)

            def pair_tasks(ia, ib):
                for tt in range(8):
                    for db in range(4):
                        yield (ia, ib, tt, db, tt == 7 and db == 3)

            def do_task(task):
                (ia, ib, tt, db, last) = task
                pb = st['dk'] % 4
                st['dk'] += 1
                pe.after(ia['t_avs'][tt // 4], ib['t_avs'][tt // 4], ia['c3'], ib['c3'], st['pd_free'][pb])
                nc.tensor.matmul(pd[pb][:, :], lhsT=av[ia['av']][:, tt * P:(tt + 1) * P],
                                 rhs=wds[ia['wd']][:, db * 512:(db + 1) * 512], start=True, stop=False)
                mm = nc.tensor.matmul(pd[pb][:, :], lhsT=av[ib['av']][:, tt * P:(tt + 1) * P],
                                      rhs=wds[ib['wd']][:, db * 512:(db + 1) * 512], start=False, stop=True)
                t_mm = pe.done(mm)
                e = ia['e']
                t_evl = dve.op(lambda: nc.vector.scalar_tensor_tensor(
                    out=yacc[:, tt, db * 512:(db + 1) * 512], in0=pd[pb][:, :],
                    scalar=gb[:, tt, e:e + 1],
                    in1=yacc[:, tt, db * 512:(db + 1) * 512], op0=ALU.mult, op1=ALU.add), t_mm, ser=False)
                st['pd_free'][pb] = t_evl
                if last:
                    for info in (ia, ib):
                        st['av_free'][info['av']] = t_mm
                        st['wd_free'][info['wd']] = t_mm
                return t_evl

            def run_tasks(gen, n):
                t = None
                for _ in range(n):
                    task = next(gen, None)
                    if task is None:
                        break
                    t = do_task(task)
                return t

            gen = iter(())
            prev = None
            t_dn = None
            for ui, (e, un) in enumerate(units):
                if e == 0:
                    for db_ in (range(4) if un == 0 else []):
                        t_bd = dma(sp, bdt[:], bd_b[e][:, db_ * 512:(db_ + 1) * 512], bd_sl, st['bd_free'])
                        t_i = None
                        for tt in range(8):
                            t_i = dve.op(lambda: nc.vector.tensor_scalar(
                                out=yacc[:, tt, db_ * 512:(db_ + 1) * 512], in0=bdt[:], scalar1=gb[:, tt, e:e + 1],
                                scalar2=None, op0=ALU.mult), t_bd, t_b)
                        st['bd_free'] = t_i
                else:
                    db_ = un // 4
                    if un % 4 == 0:
                        st['t_bd'] = dma(sp, bdt[:], bd_b[e][:, db_ * 512:(db_ + 1) * 512], bd_sl, st['bd_free'])
                    t_i = None
                    for tt in ((un % 4) * 2, (un % 4) * 2 + 1):
                        t_i = dve.op(lambda: nc.vector.scalar_tensor_tensor(
                            out=yacc[:, tt, db_ * 512:(db_ + 1) * 512], in0=bdt[:], scalar=gb[:, tt, e:e + 1],
                            in1=yacc[:, tt, db_ * 512:(db_ + 1) * 512], op0=ALU.mult, op1=ALU.add), st['t_bd'])
                    if un % 4 == 3:
                        st['bd_free'] = t_i
                    dve.after(('e', dve, dve.n))
                gu = ui % NGU
                ab = ui % NAV
                (c1, c2, c3) = w_tok.pop(ui)
                t_avs = []
                cnt = 0
                t_u = None
                for tb in range(2):
                    pe.after(c1, t_b, st['pg_free'][tb])
                    for kc in range(KC):
                        mm = nc.tensor.matmul(pg[tb][:, :], lhsT=wgs[gu][:, kc, :], rhs=hb[:, kc, tb * 512:(tb + 1) * 512],
                                              start=(kc == 0), stop=(kc == KC - 1))
                        if kc == KC - 1:
                            t_g = pe.done(mm)
                        cnt += 1
                        if cnt % 4 == 0:
                            run_tasks(gen, 1)
                    t_gc = dve.op(lambda: nc.vector.tensor_scalar(out=gcl[tb][:], in0=pg[tb][:, :],
                                                                  scalar1=bgc[:, e, un:un + 1], scalar2=LIMIT,
                                                                  op0=ALU.add, op1=ALU.min), t_g, t_b, st['tmp_free'][tb])
                    st['pg_free'][tb] = t_gc
                    t_s = act.op(lambda: nc.scalar.activation(out=sg[tb][:], in_=gcl[tb][:], func=AF.Sigmoid, scale=ALPHA),
                                 t_gc, st['sg_free'])
                    pe.after(c2, st['pu_free'][tb])
                    for kc in range(KC):
                        mm = nc.tensor.matmul(pu[tb][:, :], lhsT=wus[gu][:, kc, :], rhs=hb[:, kc, tb * 512:(tb + 1) * 512],
                                              start=(kc == 0), stop=(kc == KC - 1))
                        if kc == KC - 1:
                            t_u = pe.done(mm)
                        cnt += 1
                        if cnt % 4 == 0:
                            run_tasks(gen, 1)
                    t_ur = act.op(lambda: nc.scalar.activation(out=ucl[tb][:], in_=pu[tb][:, :], func=AF.Identity,
                                                               bias=buc[:, e, un:un + 1], scale=1.0), t_u, st['tmp_free'][tb])
                    st['pu_free'][tb] = t_ur
                    t_gs = pool.op(lambda: nc.gpsimd.tensor_tensor(out=gcl[tb][:], in0=gcl[tb][:], in1=sg[tb][:],
                                                                   op=ALU.mult), t_s)
                    st['sg_free'] = t_gs
                    pool.op(lambda: nc.gpsimd.tensor_scalar(out=ucl[tb][:], in0=ucl[tb][:], scalar1=LIMIT + 1.0,
                                                            scalar2=-LIMIT + 1.0, op0=ALU.min, op1=ALU.max), t_ur)
                    t_av = pool.op(lambda: nc.gpsimd.tensor_tensor(
                        out=av[ab][:, tb * 512:(tb + 1) * 512], in0=ucl[tb][:], in1=gcl[tb][:],
                        op=ALU.mult), st['av_free'][ab] if tb == 0 else None)
                    st['tmp_free'][tb] = t_av
                    t_avs.append(t_av)
                st['gu_free'][gu] = t_u
                info = dict(e=e, av=ab, wd=ui % NWD, t_avs=t_avs, c3=c3)
                if ui % 2 == 1:
                    run_tasks(gen, 64)
                    gen = pair_tasks(prev, info)
                else:
                    prev = info
                if ui + PRE < len(units):
                    w_tok[ui + PRE] = issue_w(ui + PRE)
            t_dn = run_tasks(gen, 64)
            barrier_all()
            ph.close()
            ph = ph0

            xr = [sb(ph, f"xr{i}", [P, D], F32) for i in range(2)]
            ld_sl = [slot("fl") for _ in range(2)]
            st_sl = [slot("fs") for _ in range(2)]
            junk = sb(ph, "junk9", [P, D], F32)
            fs = sb(ph, "fs", [P, 16], F32)
            gf_row = sb(ph, "gf_row10", [P, D], F32)
            t_gf = dma(sp, gf_row[:], gf_scr[:, :], slot("m"))
            dve.op(lambda: nc.vector.memset(fs[:], 0.0))
            st_free = [None, None]
            for tt in range(8):
                b = tt % 2
                t_ld = dma(sp, xr[b][:], x1_scr[tt * P:(tt + 1) * P, :], ld_sl[b], st_free[b])
                t_sq = act.op(lambda: nc.scalar.activation(out=junk[:], in_=yacc[:, tt, :], func=AF.Square,
                                                           accum_out=fs[:, tt:tt + 1]), t_dn)
                rstd(fs[:, 8 + tt:9 + tt], fs[:, tt:tt + 1], 1.0 / D, t_sq)
                dve.op(lambda: nc.vector.scalar_tensor_tensor(out=yacc[:, tt, :], in0=yacc[:, tt, :],
                                                              scalar=fs[:, 8 + tt:9 + tt],
                                                              in1=gf_row[:], op0=ALU.mult, op1=ALU.mult), t_gf)
                t_o = dve.op(lambda: nc.vector.tensor_tensor(out=xr[b][:], in0=yacc[:, tt, :], in1=xr[b][:], op=ALU.add),
                             t_ld)
                st_free[b] = dma(sp, out[tt * P:(tt + 1) * P, :], xr[b][:], st_sl[b], t_o)
            sp.after(st_sl[0].tok(), st_sl[1].tok())
    return nc


_CACHE = {}


def _bf(a):
    return np.ascontiguousarray(a.astype(ml_dtypes.bfloat16))


def _f(a):
    return np.ascontiguousarray(a, dtype=np.float32)


def _col(v):
    return _f(np.asarray(v).reshape(KC, P).T)


def _rep(v):
    return _f(np.broadcast_to(np.asarray(v)[None, :], (P, v.shape[-1])))


def kernel(x, c, w_ada, b_ada, g_pre_mix, w_in, w_fourier, q_norm_g, k_norm_g, g_fourier_out, g_attn_out, w_out,
           g_post_mix, g_pre_ffn, w_router, b_router, w_gate, b_gate, w_up, b_up, w_down, b_down, g_post_ffn):
    arrs = [np.asarray(a) for a in (x, c, w_ada, b_ada, g_pre_mix, w_in, w_fourier, q_norm_g, k_norm_g,
                                    g_fourier_out, g_attn_out, w_out, g_post_mix, g_pre_ffn, w_router, b_router,
                                    w_gate, b_gate, w_up, b_up, w_down, b_down, g_post_ffn)]
    (x, c, w_ada, b_ada, g_pre_mix, w_in, w_fourier, q_norm_g, k_norm_g, g_fourier_out, g_attn_out, w_out,
     g_post_mix, g_pre_ffn, w_router, b_router, w_gate, b_gate, w_up, b_up, w_down, b_down, g_post_ffn) = arrs
    if 'nc' not in _CACHE:
        _CACHE['nc'] = build()
    nc = _CACHE['nc']
    inv_freq = (1.0 / (10000.0 ** (np.arange(0, 64, 2, dtype=np.float32) / 64.0))).astype(np.float32)
    cc64 = np.arange(128, dtype=np.int64)
    ang_c = 2.0 * np.pi * ((cc64[:, None] * cc64[None, :]) % 128).astype(np.float64) / 128.0
    cs_tab = _f(np.concatenate([np.cos(ang_c) / 512.0, -np.sin(ang_c) / 512.0], axis=1))
    ident = np.eye(P, dtype=np.float32)
    shared = dict(
        w_ada=_f(w_ada[0]), b_ada=_f(b_ada[0][None, :]), gpm_col=_col(g_pre_mix[0]), gpf_col=_col(g_pre_ffn[0]),
        w_in=_f(w_in[0]), w_out=_f(w_out[0]), wf=_f(np.transpose(w_fourier[0], (1, 0, 2))),
        qg_b=_rep(q_norm_g[0]), kg_b=_rep(k_norm_g[0]),
        gmerge_b=_rep(np.concatenate([g_fourier_out[0], g_attn_out[0]])),
        gpostm_b=_rep(g_post_mix[0]), gpostf_b=_rep(g_post_ffn[0]),
        w_router=_f(np.transpose(w_router[0].reshape(KC, P, 32), (1, 0, 2))), b_router_b=_rep(b_router[0]),
        cs_tab=cs_tab, ident_in=ident,
        wg=_f(w_gate[0]), wu=_f(w_up[0]), wd=_f(w_down[0]),
        bg_col=_f(np.transpose(b_gate[0].reshape(32, KC, P), (2, 0, 1))),
        bu_col=_f(np.transpose(b_up[0].reshape(32, KC, P), (2, 0, 1))),
        bd_b=_f(np.broadcast_to(b_down[0][:, None, :], (32, P, D))),
    )
    in_maps = []
    for core in range(NCORE):
        b, half = core // 2, core % 2
        own = np.arange(half * OWN, (half + 1) * OWN)
        oth = np.arange((1 - half) * OWN, (2 - half) * OWN)
        perm = np.concatenate([own, oth])
        pos = perm.astype(np.float32)
        row_idx = np.floor(pos / 64.0).astype(np.float32)
        col_idx = (pos - row_idx * 64.0).astype(np.float32)
        ang = np.concatenate([row_idx[:, None] * inv_freq[None, :], col_idx[:, None] * inv_freq[None, :]],
                             axis=1).astype(np.float32)
        cos_t = np.cos(ang).astype(np.float32).reshape(16, P, 64).transpose(1, 0, 2)
        sin_t = np.sin(ang).astype(np.float32).reshape(16, P, 64).transpose(1, 0, 2)
        prod = (perm.astype(np.int64)[:, None] * own.astype(np.int64)[None, :]) % S
        ang_s = 2.0 * np.pi * prod.astype(np.float64) / S
        m = dict(shared)
        m.update(
            xp=_f(x[b][perm]), c_col=_col(c[b]),
            cos_t=_f(cos_t), sin_t=_f(sin_t), ctab=_f(np.cos(ang_s)), stab=_f(np.sin(ang_s)),
        )
        in_maps.append(m)
    res = run_bass_kernel_spmd(nc, in_maps, core_ids=list(range(NCORE)))
    outp = np.empty((4, S, D), np.float32)
    for core in range(NCORE):
        b, half = core // 2, core % 2
        outp[b, half * OWN:(half + 1) * OWN] = res.results[core]["out"]
    return outp
```

```python
import numpy as np
import ml_dtypes
from contextlib import ExitStack
import concourse.bass as bass
import concourse.mybir as mybir
from concourse.bass_utils import run_bass_kernel_spmd

F32 = mybir.dt.float32
BF16 = mybir.dt.bfloat16
ALU = mybir.AluOpType
AF = mybir.ActivationFunctionType
AX = mybir.AxisListType

P = 128
D = 2048
KC = 16
S = 2048
OWN = 1024
NCORE = 8
NE_OWN = 4
EPS = 1e-6
LIMIT = 7.0
ALPHA = 1.702


class Eng:
    def __init__(self, nc, eng, sem, serial):
        self.nc, self.eng, self.sem, self.n = nc, eng, sem, 0
        self.waited = {}
        self.serial = serial

    def after(self, *toks):
        for t in toks:
            if t is None:
                continue
            if t[0] == 'd':
                _, sem, val = t
                key = ('d', id(sem))
                if self.waited.get(key, 0) < val:
                    self.eng.wait_ge(sem, val)
                    self.waited[key] = val
            else:
                _, prod, n = t
                if prod is self and not self.serial:
                    continue
                key = ('e', id(prod))
                if self.waited.get(key, 0) < n:
                    self.eng.wait_ge(prod.sem, n)
                    self.waited[key] = n

    def done(self, instr):
        self.n += 1
        instr.then_inc(self.sem, 1)
        return ('e', self, self.n)

    def op(self, fn, *deps, ser=True):
        self.after(*deps)
        if self.serial and ser and self.n > 0:
            self.after(('e', self, self.n))
        return self.done(fn())


class Slot:
    def __init__(self, sem):
        self.sem, self.cnt = sem, 0

    def add(self, instr):
        self.cnt += 16
        instr.then_inc(self.sem, 16)
        return ('d', self.sem, self.cnt)

    def tok(self):
        return ('d', self.sem, self.cnt)


def build(stage=99):
    nc = bass.Bass("TRN2", target_bir_lowering=False)

    def din(name, shape, dt=F32):
        return nc.dram_tensor(name, list(shape), dt, kind="ExternalInput").ap()

    def dscr(name, shape, dt):
        return nc.dram_tensor(name, list(shape), dt, kind="Internal").ap()

    xp = din("xp", [S, D])
    c_col = din("c_col", [P, KC])
    w_ada = din("w_ada", [D, 6 * D])
    b_ada = din("b_ada", [1, 6 * D])
    gpm_col = din("gpm_col", [P, KC])
    gpf_col = din("gpf_col", [P, KC])
    w_in = din("w_in", [D, 3072])
    w_out = din("w_out", [D, D])
    wf = din("wf", [P, 4, P])
    qg_b = din("qg_b", [P, P])
    kg_b = din("kg_b", [P, P])
    gmerge_b = din("gmerge_b", [P, D])
    gpostm_b = din("gpostm_b", [P, D])
    gpostf_b = din("gpostf_b", [P, D])
    w_router = din("w_router", [P, KC, 32])
    b_router_b = din("b_router_b", [P, 32])
    wg = din("wg", [32, D, D])
    wu = din("wu", [32, D, D])
    wd = din("wd", [32, D, D])
    bg_col = din("bg_col", [P, 32, KC])
    bu_col = din("bu_col", [P, 32, KC])
    bd_b = din("bd_b", [32, P, D])
    cos_t = din("cos_t", [P, 16, 64])
    sin_t = din("sin_t", [P, 16, 64])
    ctab_f = din("ctab", [S, OWN])
    stab_f = din("stab", [S, OWN])
    cs_tab_f = din("cs_tab", [P, 256])
    ident_in = din("ident_in", [P, P])
    out = nc.dram_tensor("out", [OWN, D], F32, kind="ExternalOutput").ap()

    w_in_bf = dscr("w_in_bf", [D, 3072], BF16)
    ctab = dscr("ctab_bf", [S, OWN], BF16)
    stab = dscr("stab_bf", [S, OWN], BF16)
    cs_tab = dscr("cs_tab_bf", [P, 256], BF16)
    w_out_bf = dscr("w_out_bf", [D, D], BF16)
    x1_scr = dscr("x1_scr", [OWN, D], F32)
    ht_send = dscr("ht_send", [D, OWN], BF16)
    g_send = dscr("g_send", [OWN, 32], F32)
    gm_scr = dscr("gm_scr", [P, D], F32)
    gf_scr = dscr("gf_scr", [P, D], F32)

    with ExitStack() as top:
        nsem = [0]

        def newsem(name):
            nsem[0] += 1
            return top.enter_context(nc.semaphore(f"{name}_{nsem[0]}"))

        def slot(name="dq"):
            return Slot(newsem(name))

        pe = Eng(nc, nc.tensor, newsem("pe"), False)
        act = Eng(nc, nc.scalar, newsem("act"), True)
        dve = Eng(nc, nc.vector, newsem("dve"), True)
        pool = Eng(nc, nc.gpsimd, newsem("pool"), True)
        sp = Eng(nc, nc.sync, newsem("sp"), False)

        def sb(ctx, name, shape, dt):
            nsem[0] += 1
            return ctx.enter_context(nc.sbuf_tensor(f"{name}_{nsem[0]}", list(shape), dt))

        def ps(ctx, name, shape, dt=F32):
            nsem[0] += 1
            return ctx.enter_context(nc.psum_tensor(f"{name}_{nsem[0]}", list(shape), dt))

        def dma(q, out_ap, in_ap, sl, *deps):
            q.after(*deps)
            return sl.add(q.eng.dma_start(out=out_ap, in_=in_ap))

        def rstd(out_ap, in_ap, inv_n, *deps):
            t_a = act.op(lambda: nc.scalar.activation(out=out_ap, in_=in_ap, func=AF.Sqrt, scale=inv_n,
                                                      bias=eps_t[:, 0:1]), *deps)
            return dve.op(lambda: nc.vector.reciprocal(out=out_ap, in_=out_ap), t_a)

        def barrier(*extra):
            toks = [('e', pe, pe.n), ('e', act, act.n), ('e', dve, dve.n)] + list(extra)
            for en in (pe, act, dve, sp):
                en.after(*toks)

        cast_w_in = slot("cwi")
        cast_w_out = slot("cwo")
        for h in range(2):
            cast_w_in.add(nc.gpsimd.dma_start(out=w_in_bf[:, h * 1536:(h + 1) * 1536],
                                              in_=w_in[:, h * 1536:(h + 1) * 1536]))
        cast_tab = slot("ctb")
        cast_tab.add(nc.gpsimd.dma_start(out=cs_tab[:, :], in_=cs_tab_f[:, :]))
        cast_tab.add(nc.gpsimd.dma_start(out=ctab[:, :], in_=ctab_f[:, :]))
        cast_tab.add(nc.gpsimd.dma_start(out=stab[:, :], in_=stab_f[:, :]))
        cast_w_out.add(nc.gpsimd.dma_start(out=w_out_bf[:, :], in_=w_out[:, :]))

        ident_f = sb(top, "ident_f", [P, P], F32)
        ident_b = sb(top, "ident_b", [P, P], BF16)
        ones_f = sb(top, "ones_f", [P, P], F32)
        modcol = sb(top, "modcol", [P, 64], F32)
        a_m = sb(top, "a_m", [P, KC], F32)
        a_f = sb(top, "a_f", [P, KC], F32)
        t_id = dma(sp, ident_f[:], ident_in[:, :], slot("m"))
        t_idb = dve.op(lambda: nc.vector.tensor_copy(out=ident_b[:], in_=ident_f[:]), t_id)
        t_ones = dve.op(lambda: nc.vector.memset(ones_f[:], 1.0))
        eps_t = sb(top, "eps_t", [P, 1], F32)
        dve.op(lambda: nc.vector.memset(eps_t[:], EPS))

        with ExitStack() as ph:
            NWA = 3
            wa = [sb(ph, f"wa{i}", [P, KC, 512], F32) for i in range(NWA)]
            wa_sl = [slot("wa") for _ in range(NWA)]
            modrow = sb(ph, "modrow", [1, 6 * D], F32)
            cc = sb(ph, "cc", [P, KC], F32)
            sc = sb(ph, "sc", [P, KC], F32)
            gpm = sb(ph, "gpm", [P, KC], F32)
            gpf = sb(ph, "gpf", [P, KC], F32)
            gpb = sb(ph, "gpb", [P, D], F32)
            gm_row = sb(ph, "gm_row", [P, D], F32)
            gf_row = sb(ph, "gf_row", [P, D], F32)
            ps_row = [ps(ph, f"ps_row{i}", [P, 512]) for i in range(2)]
            ps_col = ps(ph, "ps_col", [P, 64])
            ps_bc = [ps(ph, f"ps_bc{i}", [P, 512]) for i in range(2)]
            misc = slot("m")
            t_c = dma(sp, cc[:], c_col[:, :], misc)
            t_c = dma(sp, modrow[:], b_ada[:, :], misc)
            t_c = dma(sp, gpm[:], gpm_col[:, :], misc)
            t_c = dma(sp, gpf[:], gpf_col[:, :], misc)
            t_sg = act.op(lambda: nc.scalar.activation(out=sc[:], in_=cc[:], func=AF.Sigmoid), t_c)
            t_sc = dve.op(lambda: nc.vector.tensor_tensor(out=sc[:], in0=sc[:], in1=cc[:], op=ALU.mult), t_sg)
            w_ada_v = w_ada.rearrange("(kc p) n -> p kc n", p=P)
            wa_free = [None] * NWA
            psr_free = [None, None]
            t_row = None
            t_lds = {}
            for nb in range(NWA - 1):
                t_lds[nb] = dma(sp, wa[nb % NWA][:], w_ada_v[:, :, nb * 512:(nb + 1) * 512], wa_sl[nb % NWA])
            for nb in range(24):
                b = nb % 2
                wb_ = nb % NWA
                nx = nb + NWA - 1
                if nx < 24:
                    t_lds[nx] = dma(sp, wa[nx % NWA][:], w_ada_v[:, :, nx * 512:(nx + 1) * 512], wa_sl[nx % NWA],
                                    wa_free[nx % NWA])
                pe.after(t_lds.pop(nb), t_sc, psr_free[b])
                for kc in range(KC):
                    mm = nc.tensor.matmul(ps_row[b][0:1, :], lhsT=sc[:, kc:kc + 1], rhs=wa[wb_][:, kc, :],
                                          start=(kc == 0), stop=(kc == KC - 1))
                t_mm = pe.done(mm)
                wa_free[wb_] = t_mm
                t_row = dve.op(lambda: nc.vector.tensor_tensor(
                    out=modrow[0:1, nb * 512:(nb + 1) * 512], in0=ps_row[b][0:1, :],
                    in1=modrow[0:1, nb * 512:(nb + 1) * 512], op=ALU.add), t_mm, t_c)
                psr_free[b] = t_row
            pe.after(t_row, t_ones)
            for v, j in enumerate((0, 1, 3, 4)):
                for c in range(KC):
                    mm = nc.tensor.matmul(ps_col[:, v * 16 + c:v * 16 + c + 1],
                                          lhsT=modrow[0:1, j * D + c * P:j * D + (c + 1) * P],
                                          rhs=ones_f[0:1, 0:1], start=True, stop=True)
            t_mm = pe.done(mm)
            t_mc = dve.op(lambda: nc.vector.tensor_copy(out=modcol[:], in_=ps_col[:]), t_mm)
            dve.op(lambda: nc.vector.scalar_tensor_tensor(out=a_m[:], in0=modcol[:, 16:32], scalar=1.0,
                                                          in1=gpm[:], op0=ALU.add, op1=ALU.mult))
            t_am = dve.op(lambda: nc.vector.scalar_tensor_tensor(out=a_f[:], in0=modcol[:, 48:64], scalar=1.0,
                                                                 in1=gpf[:], op0=ALU.add, op1=ALU.mult))
            bc_free = [None, None]
            k = 0
            for (j, dst, gsrc) in ((2, gm_row, gpostm_b), (5, gf_row, gpostf_b)):
                t_g = dma(sp, gpb[:], gsrc[:, :], slot("m"), t_am if k == 0 else t_bc)
                for nb in range(4):
                    b = k % 2
                    pe.after(bc_free[b])
                    mm = nc.tensor.matmul(ps_bc[b][:, :], lhsT=ones_f[0:1, :],
                                          rhs=modrow[0:1, j * D + nb * 512:j * D + (nb + 1) * 512],
                                          start=True, stop=True)
                    t_mm = pe.done(mm)
                    t_bc = dve.op(lambda: nc.vector.tensor_tensor(out=dst[:, nb * 512:(nb + 1) * 512],
                                                                  in0=ps_bc[b][:, :],
                                                                  in1=gpb[:, nb * 512:(nb + 1) * 512],
                                                                  op=ALU.mult), t_mm, t_g)
                    bc_free[b] = t_bc
                    k += 1
            g_sl = slot("gs")
            dma(sp, gm_scr[:, :], gm_row[:], g_sl, t_bc)
            t_gs = dma(sp, gf_scr[:, :], gf_row[:], g_sl)
            barrier(t_gs)

        with ExitStack() as mx:
            mergedT = sb(mx, "mergedT", [P, KC, OWN], BF16)
            cosb = sb(mx, "cosb", [P, 16, 64], F32)
            sinb = sb(mx, "sinb", [P, 16, 64], F32)
            qgb = sb(mx, "qgb", [P, P], F32)
            kgb = sb(mx, "kgb", [P, P], F32)
            kvq = mx.enter_context(ExitStack())
            kT = sb(kvq, "kT", [P, 4, S], BF16)
            vaug = sb(kvq, "vaug", [P, 16, 4, 132], BF16)
            qT = sb(kvq, "qT", [P, 4, 8, 384], BF16)
            misc = slot("m")
            t_tab = dma(sp, cosb[:], cos_t[:, :, :], misc)
            t_tab = dma(sp, sinb[:], sin_t[:, :, :], misc)
            t_tab = dma(sp, qgb[:], qg_b[:, :], misc)
            t_tab = dma(sp, kgb[:], kg_b[:, :], misc)
            t_v1 = dve.op(lambda: nc.vector.memset(vaug[:, :, :, 128:129], 1.0))

            with ExitStack() as zsc:
                zs = sb(zsc, "zs", [P, 16, 4, 256], BF16)
                cst = sb(zsc, "cst", [P, 256], BF16)
                t_cs = dma(sp, cst[:], cs_tab[:, :], slot("m"), cast_tab.tok())
                w_in_v = w_in_bf.rearrange("(kc p) n -> p kc n", p=P)
                for th in range(2):
                  with ExitStack() as pj:
                    hT = sb(pj, "hT", [P, KC, OWN], BF16)
                    with ExitStack() as ph:
                        xt = [sb(ph, f"xt{i}", [P, D], F32) for i in range(2)]
                        xt_sl = [slot("xt") for _ in range(2)]
                        xs = [sb(ph, f"xs{i}", [P, D], BF16) for i in range(2)]
                        junk = sb(ph, "junk", [P, D], F32)
                        ss = sb(ph, "ss", [P, 8], F32)
                        rs = sb(ph, "rs", [P, 8], F32)
                        dve.op(lambda: nc.vector.memset(ss[:], 0.0))
                        pst = [ps(ph, f"pst{i}", [P, 512], BF16) for i in range(4)]
                        xt_free = [None, None]
                        xs_free = [None, None]
                        pst_free = [None] * 4
                        kk = 0
                        t_ev = None
                        for i in range(8):
                            b = i % 2
                            tile = th * 8 + i
                            t_ld = dma(sp, xt[b][:], xp[tile * P:(tile + 1) * P, :], xt_sl[b], xt_free[b])
                            t_sq = act.op(lambda: nc.scalar.activation(out=junk[:], in_=xt[b][:], func=AF.Square,
                                                                       accum_out=ss[:, i:i + 1]), t_ld)
                            t_rs = rstd(rs[:, i:i + 1], ss[:, i:i + 1], 1.0 / D, t_sq)
                            t_xs = act.op(lambda: nc.scalar.activation(out=xs[b][:], in_=xt[b][:],
                                                                       func=AF.Identity, scale=rs[:, i:i + 1]),
                                          t_rs, xs_free[b])
                            xt_free[b] = t_xs
                            t_tr = None
                            for q4 in range(4):
                                pb = kk % 4
                                kk += 1
                                pe.after(t_xs, t_idb, pst_free[pb])
                                for c4 in range(4):
                                    dc = q4 * 4 + c4
                                    mm = nc.tensor.transpose(out=pst[pb][:, c4 * P:(c4 + 1) * P],
                                                             in_=xs[b][:, dc * P:(dc + 1) * P], identity=ident_b[:])
                                t_tr = pe.done(mm)
                                for c4 in range(4):
                                    dc = q4 * 4 + c4
                                    t_ev = act.op(lambda: nc.scalar.activation(
                                        out=hT[:, dc, i * P:(i + 1) * P], in_=pst[pb][:, c4 * P:(c4 + 1) * P],
                                        func=AF.Identity, scale=a_m[:, dc:dc + 1], bias=modcol[:, dc:dc + 1]), t_tr,
                                        ser=(c4 == 0))
                                pst_free[pb] = t_ev
                            xs_free[b] = t_tr
                        t_hT = t_ev
                    barrier()

                    with ExitStack() as ph:
                        wb = [sb(ph, f"wb{i}", [P, KC, 512], BF16) for i in range(1)]
                        wb_sl = [slot("wb") for _ in range(1)]
                        fT = sb(ph, "fT", [P, 4, OWN], BF16)
                        sq = sb(ph, "sq", [P, 512], F32)
                        t1 = sb(ph, "t1", [P, 512], F32)
                        m1 = sb(ph, "m1", [P, 256], F32)
                        m2 = sb(ph, "m2", [P, 256], F32)
                        qn = [sb(ph, f"qn{i}", [P, 512], BF16) for i in range(2)]
                        ssh = sb(ph, "ssh", [P, 4], F32)
                        rsh = sb(ph, "rsh", [P, 4], F32)
                        pp = [ps(ph, f"pp{i}", [P, 512]) for i in range(3)]
                        pz = [ps(ph, f"pz{i}", [P, 1024]) for i in range(1)]
                        ptq = [ps(ph, f"ptq{i}", [P, 512], BF16) for i in range(2)]
                        wb_free = [None, None]
                        pp_free = [None] * 3
                        ptq_free = [None, None]
                        qn_free = [None, None]
                        ppk = [0]
                        qk = [0]
                        order = [0, 4, 5] + ([1, 2, 3] if th == 0 else [])

                        def normrope(psrc, t_mm, gtile, tt, dst_bf, dst_free):
                            t_a = act.op(lambda: nc.scalar.activation(out=sq[:], in_=psrc[:, :], func=AF.Square), t_mm)
                            t_rd = dve.op(lambda: nc.vector.tensor_reduce(out=ssh[:], in_=sq[:].rearrange("p (h d) -> p h d", h=4),
                                                                          axis=AX.X, op=ALU.add), t_a)
                            rstd(rsh[:], ssh[:], 1.0 / 128, t_rd)
                            t13 = t1[:].rearrange("p (h d) -> p h d", h=4)
                            dve.op(lambda: nc.vector.tensor_tensor(
                                out=t13, in0=psrc[:, :].rearrange("p (h d) -> p h d", h=4),
                                in1=rsh[:].unsqueeze(2).to_broadcast([P, 4, P]), op=ALU.mult))
                            dve.op(lambda: nc.vector.tensor_tensor(
                                out=t13, in0=t13, in1=gtile[:].unsqueeze(1).to_broadcast([P, 4, P]), op=ALU.mult), t_tab)
                            t14 = t1[:].rearrange("p (h d two) -> p h d two", h=4, two=2)
                            x0 = t14[:, :, :, 0]
                            x1 = t14[:, :, :, 1]
                            cb = cosb[:, tt, :].unsqueeze(1).to_broadcast([P, 4, 64])
                            sbn = sinb[:, tt, :].unsqueeze(1).to_broadcast([P, 4, 64])
                            m13 = m1[:].rearrange("p (h d) -> p h d", h=4)
                            m23 = m2[:].rearrange("p (h d) -> p h d", h=4)
                            d4 = dst_bf[:].rearrange("p (h d two) -> p h d two", h=4, two=2)
                            dve.op(lambda: nc.vector.tensor_tensor(out=m13, in0=x0, in1=cb, op=ALU.mult))
                            dve.op(lambda: nc.vector.tensor_tensor(out=m23, in0=x1, in1=sbn, op=ALU.mult), ser=False)
                            dve.op(lambda: nc.vector.tensor_tensor(out=d4[:, :, :, 0], in0=m13, in1=m23, op=ALU.subtract),
                                   dst_free)
                            dve.op(lambda: nc.vector.tensor_tensor(out=m13, in0=x0, in1=sbn, op=ALU.mult))
                            dve.op(lambda: nc.vector.tensor_tensor(out=m23, in0=x1, in1=cb, op=ALU.mult), ser=False)
                            return dve.op(lambda: nc.vector.tensor_tensor(out=d4[:, :, :, 1], in0=m13, in1=m23, op=ALU.add))

                        for bi, cb_ in enumerate(order):
                            b = 0
                            t_w = dma(sp, wb[b][:], w_in_v[:, :, cb_ * 512:(cb_ + 1) * 512], wb_sl[b],
                                      wb_free[b], cast_w_in.tok())
                            t_last = None
                            if cb_ == 0:
                                t_ev = None
                                for g in range(4):
                                    for tb in range(2):
                                        pb = ppk[0] % 3
                                        ppk[0] += 1
                                        pe.after(t_w, t_hT, pp_free[pb])
                                        for kc in range(KC):
                                            mm = nc.tensor.matmul(pp[pb][:, :], lhsT=wb[b][:, kc, g * P:(g + 1) * P],
                                                                  rhs=hT[:, kc, tb * 512:(tb + 1) * 512],
                                                                  start=(kc == 0), stop=(kc == KC - 1))
                                        t_mm = pe.done(mm)
                                        t_ev = act.op(lambda: nc.scalar.copy(out=fT[:, g, tb * 512:(tb + 1) * 512],
                                                                             in_=pp[pb][:, :]), t_mm)
                                        pp_free[pb] = t_ev
                                t_last = t_mm
                                pz_free = None
                                for ttl in range(8):
                                    tile = th * 8 + ttl
                                    pe.after(t_ev, t_cs, pz_free)
                                    for g in range(4):
                                        mm = nc.tensor.matmul(pz[0][:, g * 256:(g + 1) * 256],
                                                              lhsT=fT[:, g, ttl * P:(ttl + 1) * P], rhs=cst[:, :],
                                                              start=True, stop=True)
                                    t_mm = pe.done(mm)
                                    t_z = dve.op(lambda: nc.vector.tensor_copy(
                                        out=zs[:, tile, :, :].rearrange("p g c -> p (g c)"), in_=pz[0][:, :]), t_mm)
                                    pz_free = t_z
                            else:
                                for ttl in range(8):
                                    tile = th * 8 + ttl
                                    pb = ppk[0] % 3
                                    ppk[0] += 1
                                    pe.after(t_w, t_hT, pp_free[pb])
                                    for kc in range(KC):
                                        mm = nc.tensor.matmul(pp[pb][:, :], lhsT=hT[:, kc, ttl * P:(ttl + 1) * P],
                                                              rhs=wb[b][:, kc, :], start=(kc == 0), stop=(kc == KC - 1))
                                    t_mm = pe.done(mm)
                                    t_last = t_mm
                                    if cb_ == 5:
                                        t_ev = act.op(lambda: nc.scalar.copy(
                                            out=vaug[:, tile, :, 0:128],
                                            in_=pp[pb][:, :].rearrange("p (h d) -> p h d", h=4)), t_mm, t_v1)
                                        pp_free[pb] = t_ev
                                        continue
                                    qb = qk[0] % 2
                                    qk[0] += 1
                                    t_nr = normrope(pp[pb], t_mm, kgb if cb_ == 4 else qgb, tile, qn[qb], qn_free[qb])
                                    pp_free[pb] = t_nr
                                    pe.after(t_nr, ptq_free[qb])
                                    for h in range(4):
                                        mm = nc.tensor.transpose(out=ptq[qb][:, h * P:(h + 1) * P],
                                                                 in_=qn[qb][:, h * P:(h + 1) * P], identity=ident_b[:])
                                    t_tr = pe.done(mm)
                                    qn_free[qb] = t_tr
                                    if cb_ == 4:
                                        t_ev = act.op(lambda: nc.scalar.copy(
                                            out=kT[:, :, tile * P:(tile + 1) * P],
                                            in_=ptq[qb][:, :].rearrange("p (h t) -> p h t", h=4)), t_tr)
                                    else:
                                        t_ev = None
                                        for hh in range(4):
                                            h = 4 * (cb_ - 1) + hh
                                            t_ev = act.op(lambda: nc.scalar.copy(
                                                out=qT[:, h // 3, ttl, (h % 3) * P:(h % 3 + 1) * P],
                                                in_=ptq[qb][:, hh * P:(hh + 1) * P]), t_tr, ser=(hh == 0))
                                    ptq_free[qb] = t_ev
                            wb_free[b] = t_last
                    barrier()
                t_proj = []

                with ExitStack() as ph:
                    ct = sb(ph, "ct", [P, KC, 512], BF16)
                    st = sb(ph, "st", [P, KC, 512], BF16)
                    tab_sl = slot("tab")
                    yts = sb(ph, "yts", [P, 4, OWN], BF16)
                    wff = sb(ph, "wff", [P, 4, P], F32)
                    wfb = sb(ph, "wfb", [P, 4, P], BF16)
                    gmb = sb(ph, "gmb", [P, 512], F32)
                    junk = sb(ph, "junk4", [P, 512], F32)
                    fss = sb(ph, "fss", [P, 8], F32)
                    frs = sb(ph, "frs", [P, 8], F32)
                    dve.op(lambda: nc.vector.memset(fss[:], 0.0))
                    mtok = [sb(ph, f"mtok{i}", [P, 512], BF16) for i in range(2)]
                    py = [ps(ph, f"py{i}", [P, 512]) for i in range(2)]
                    pf = [ps(ph, f"pf{i}", [P, 512]) for i in range(2)]
                    ptm = [ps(ph, f"ptm{i}", [P, 512], BF16) for i in range(2)]
                    misc = slot("m")
                    t_wf = dma(sp, wff[:], wf[:, :, :], misc)
                    t_wf = dma(sp, gmb[:], gmerge_b[:, 0:512], misc)
                    t_wfb = dve.op(lambda: nc.vector.tensor_copy(out=wfb[:], in_=wff[:]), t_wf)
                    ctv = ctab.rearrange("(tc p) s -> p tc s", p=P)
                    stv = stab.rearrange("(tc p) s -> p tc s", p=P)
                    tab_free = None
                    py_free = [None, None]
                    k = 0
                    for sbk in range(2):
                        t_t = dma(sp, ct[:], ctv[:, :, sbk * 512:(sbk + 1) * 512], tab_sl, tab_free, cast_tab.tok())
                        t_t = dma(sp, st[:], stv[:, :, sbk * 512:(sbk + 1) * 512], tab_sl)
                        for g in range(4):
                            b = k % 2
                            k += 1
                            pe.after(t_t, py_free[b])
                            for tc in range(KC):
                                nc.tensor.matmul(py[b][:, :], lhsT=zs[:, tc, g, 0:128], rhs=ct[:, tc, :],
                                                 start=(tc == 0), stop=False)
                                mm = nc.tensor.matmul(py[b][:, :], lhsT=zs[:, tc, g, 128:256], rhs=st[:, tc, :],
                                                      start=False, stop=(tc == KC - 1))
                            t_mm = pe.done(mm)
                            t_ev = act.op(lambda: nc.scalar.copy(out=yts[:, g, sbk * 512:(sbk + 1) * 512], in_=py[b][:, :]),
                                          t_mm)
                            py_free[b] = t_ev
                        tab_free = t_mm
                    t_y = t_ev
                    pf_free = [None, None]
                    ptm_free = [None, None]
                    mt_free = [None, None]
                    for tt in range(8):
                        b = tt % 2
                        pe.after(t_y, t_wfb, pf_free[b])
                        for g in range(4):
                            mm = nc.tensor.matmul(pf[b][:, g * P:(g + 1) * P], lhsT=yts[:, g, tt * P:(tt + 1) * P],
                                                  rhs=wfb[:, g, :], start=True, stop=True)
                        t_mm = pe.done(mm)
                        t_sq = act.op(lambda: nc.scalar.activation(out=junk[:], in_=pf[b][:, :], func=AF.Square,
                                                                   accum_out=fss[:, tt:tt + 1]), t_mm)
                        rstd(frs[:, tt:tt + 1], fss[:, tt:tt + 1], 1.0 / 512, t_sq)
                        t_m = dve.op(lambda: nc.vector.scalar_tensor_tensor(
                            out=mtok[b][:], in0=pf[b][:, :], scalar=frs[:, tt:tt + 1], in1=gmb[:],
                            op0=ALU.mult, op1=ALU.mult), mt_free[b])
                        pf_free[b] = t_m
                        pe.after(t_m, ptm_free[b], t_idb)
                        for g in range(4):
                            mm = nc.tensor.transpose(out=ptm[b][:, g * P:(g + 1) * P], in_=mtok[b][:, g * P:(g + 1) * P],
                                                     identity=ident_b[:])
                        t_tr = pe.done(mm)
                        mt_free[b] = t_tr
                        t_ev = act.op(lambda: nc.scalar.copy(
                            out=mergedT[:, 0:4, tt * P:(tt + 1) * P],
                            in_=ptm[b][:, :].rearrange("p (g t) -> p g t", g=4)), t_tr)
                        ptm_free[b] = t_ev
                    t_four = []
                barrier()


            with ExitStack() as ph:
                pt = [sb(ph, f"pt{i}", [P, 16, 384], BF16) for i in range(2)]
                ao = sb(ph, "ao", [P, 1536], F32)
                aob = sb(ph, "aob", [P, 1536], BF16)
                gab = sb(ph, "gab", [P, 1536], F32)
                junk = sb(ph, "junk5", [P, 1536], F32)
                rec = sb(ph, "rec", [P, 4], F32)
                ass = sb(ph, "ass", [P, 8], F32)
                ars = sb(ph, "ars", [P, 8], F32)
                dve.op(lambda: nc.vector.memset(ass[:], 0.0))
                pss = [ps(ph, f"pss{i}", [P, 512]) for i in range(4)]
                po = [ps(ph, f"po{i}", [P, 512]) for i in range(2)]
                pta = ps(ph, "pta", [P, 1536], BF16)
                t_ga = dma(sp, gab[:], gmerge_b[:, 512:2048], slot("m"))
                pss_free = [None] * 4
                pt_free = [None, None]
                po_free = [None, None]
                pta_free = None
                aob_free = None
                ao_free = None
                sk = 0
                u = 0
                scale = float(128 ** -0.5)
                stt = dict(pta_free=None, aob_free=None, ao_free=None, t_aolast=None)

                def emit_qk(qt, g, pb):
                    nonlocal sk
                    t_exp = None
                    for kt in range(16):
                        sbn_ = sk % 4
                        sk += 1
                        pe.after(pss_free[sbn_])
                        mm = nc.tensor.matmul(pss[sbn_][:, 0:384], lhsT=kT[:, g, kt * P:(kt + 1) * P],
                                              rhs=qT[:, g, qt, :], start=True, stop=True)
                        t_mm = pe.done(mm)
                        t_exp = act.op(lambda: nc.scalar.activation(out=pt[pb][:, kt, :], in_=pss[sbn_][:, 0:384],
                                                                    func=AF.Exp, scale=scale),
                                       t_mm, pt_free[pb] if kt == 0 else None, ser=(kt == 0))
                        pss_free[sbn_] = t_exp
                    return t_exp

                def emit_pv(qt, g, pb, t_exp):
                    pe.after(t_exp, po_free[pb])
                    for j in range(3):
                        for kt in range(16):
                            mm = nc.tensor.matmul(po[pb][:, j * 132:j * 132 + 129], lhsT=pt[pb][:, kt, j * P:(j + 1) * P],
                                                  rhs=vaug[:, kt, g, 0:129], start=(kt == 0), stop=(kt == 15))
                    t_pv = pe.done(mm)
                    pt_free[pb] = t_pv
                    po3 = po[pb][:, 0:396].rearrange("p (j c) -> p j c", j=3)
                    dve.op(lambda: nc.vector.reciprocal(out=rec[:, 0:3], in_=po3[:, :, 128]), t_pv)
                    t_ao = dve.op(lambda: nc.vector.tensor_tensor(
                        out=ao[:, g * 384:(g + 1) * 384].rearrange("p (j c) -> p j c", j=3),
                        in0=po3[:, :, 0:128], in1=rec[:, 0:3].unsqueeze(2).to_broadcast([P, 3, P]),
                        op=ALU.mult), stt['ao_free'] if g == 0 else None)
                    po_free[pb] = t_ao
                    if g < 3:
                        return
                    t_sq = act.op(lambda: nc.scalar.activation(out=junk[:], in_=ao[:], func=AF.Square,
                                                               accum_out=ass[:, qt:qt + 1]), t_ao)
                    rstd(ars[:, qt:qt + 1], ass[:, qt:qt + 1], 1.0 / 1536, t_sq)
                    t_ab = dve.op(lambda: nc.vector.scalar_tensor_tensor(
                        out=aob[:], in0=ao[:], scalar=ars[:, qt:qt + 1], in1=gab[:], op0=ALU.mult, op1=ALU.mult),
                        stt['aob_free'], t_ga)
                    stt['ao_free'] = t_ab
                    pe.after(t_ab, stt['pta_free'])
                    for c in range(12):
                        mm = nc.tensor.transpose(out=pta[:, c * P:(c + 1) * P], in_=aob[:, c * P:(c + 1) * P],
                                                 identity=ident_b[:])
                    t_tr = pe.done(mm)
                    stt['aob_free'] = t_tr
                    t_ev = act.op(lambda: nc.scalar.copy(
                        out=mergedT[:, 4:16, qt * P:(qt + 1) * P],
                        in_=pta[:, :].rearrange("p (c t) -> p c t", c=12)), t_tr)
                    stt['pta_free'] = t_ev

                prev = None
                for qt in range(8):
                    for g in range(4):
                        pb = u % 2
                        u += 1
                        t_exp = emit_qk(qt, g, pb)
                        if prev is not None:
                            emit_pv(*prev)
                        prev = (qt, g, pb, t_exp)
                emit_pv(*prev)
                t_attn = []
                barrier()
            kvq.close()

            with ExitStack() as ph:
                wo = sb(ph, "wo", [P, KC, D], BF16)
                wo_sl = slot("wo")
                gm_row = sb(ph, "gm_row6", [P, D], F32)
                t_gm = dma(sp, gm_row[:], gm_scr[:, :], slot("m"))
                wr = sb(ph, "wr", [P, KC, 32], F32)
                brb = sb(ph, "brb", [P, 32], F32)
                xo = [sb(ph, f"xo{i}", [P, D], F32) for i in range(2)]
                xo_sl = [slot("xo") for _ in range(2)]
                x1 = [sb(ph, f"x1_{i}", [P, D], F32) for i in range(2)]
                x1_sl = [slot("x1s") for _ in range(2)]
                xs2 = sb(ph, "xs2", [P, D], F32)
                junk = sb(ph, "junk6", [P, D], F32)
                h2f = sb(ph, "h2f", [P, KC, P], F32)
                h2b = [sb(ph, f"h2b{i}", [P, KC, P], BF16) for i in range(2)]
                h2_sl = [slot("h2s") for _ in range(2)]
                sm = sb(ph, "sm", [P, 16], F32)
                lg = sb(ph, "lg", [P, 32], F32)
                ex = sb(ph, "ex", [P, 32], F32)
                msk = sb(ph, "msk", [P, 32], F32)
                top8 = sb(ph, "top8", [P, 8], F32)
                gt = [sb(ph, f"gt{i}", [P, 32], F32) for i in range(2)]
                gt_sl = [slot("gts") for _ in range(2)]
                pmb = [ps(ph, f"pm{i}", [P, D]) for i in range(2)]
                w_out_v = w_out_bf.rearrange("(kc p) n -> p kc n", p=P)
                t_wo = None
                for h in range(4):
                    t_wo = dma(sp, wo[:, :, h * 512:(h + 1) * 512], w_out_v[:, :, h * 512:(h + 1) * 512], wo_sl,
                               cast_w_out.tok(), *t_attn)
                misc = slot("m")
                t_wr = dma(sp, wr[:], w_router[:, :, :], misc)
                t_wr = dma(sp, brb[:], b_router_b[:, :], misc)
                xo_free = [None, None]
                x1_free = [[], []]
                h2b_free = [None, None]
                gt_free = [None, None]
                pm_free = [None, None]
                t_op = {}

                def outproj(tt):
                    pm = pmb[tt % 2]
                    pe.after(t_wo, pm_free[tt % 2])
                    for db in range(4):
                        for fc in range(KC):
                            mm = nc.tensor.matmul(pm[:, db * 512:(db + 1) * 512], lhsT=mergedT[:, fc, tt * P:(tt + 1) * P],
                                                  rhs=wo[:, fc, db * 512:(db + 1) * 512],
                                                  start=(fc == 0), stop=(fc == KC - 1))
                    t_op[tt] = pe.done(mm)

                outproj(0)
                for tt in range(8):
                    b = tt % 2
                    pm = pmb[b]
                    t_xo = dma(sp, xo[b][:], xp[tt * P:(tt + 1) * P, :], xo_sl[b], xo_free[b])
                    if tt + 1 < 8:
                        outproj(tt + 1)
                    t_mm = t_op[tt]
                    t_z = dve.op(lambda: nc.vector.memset(sm[:], 0.0))
                    for db in range(4):
                        t_sq = act.op(lambda: nc.scalar.activation(out=junk[:, db * 512:(db + 1) * 512],
                                                                   in_=pm[:, db * 512:(db + 1) * 512], func=AF.Square,
                                                                   accum_out=sm[:, db:db + 1]), t_mm, t_z, ser=(db == 0))
                    t_rd = dve.op(lambda: nc.vector.tensor_reduce(out=sm[:, 4:5], in_=sm[:, 0:4], axis=AX.X, op=ALU.add), t_sq)
                    rstd(sm[:, 5:6], sm[:, 4:5], 1.0 / D, t_rd)
                    t_a = None
                    for db in range(4):
                        t_a = dve.op(lambda: nc.vector.scalar_tensor_tensor(
                            out=x1[b][:, db * 512:(db + 1) * 512], in0=pm[:, db * 512:(db + 1) * 512],
                            scalar=sm[:, 5:6], in1=gm_row[:, db * 512:(db + 1) * 512], op0=ALU.mult, op1=ALU.mult),
                            t_gm, *(x1_free[b] if db == 0 else []), ser=(db == 0))
                    t_x1 = dve.op(lambda: nc.vector.tensor_tensor(out=x1[b][:], in0=x1[b][:], in1=xo[b][:], op=ALU.add),
                                  t_xo)
                    xo_free[b] = t_x1
                    t_st = dma(sp, x1_scr[tt * P:(tt + 1) * P, :], x1[b][:], x1_sl[b], t_x1)
                    t_sq = act.op(lambda: nc.scalar.activation(out=junk[:], in_=x1[b][:], func=AF.Square,
                                                               accum_out=sm[:, 8:9]), t_x1)
                    t_r = rstd(sm[:, 9:10], sm[:, 8:9], 1.0 / D, t_sq)
                    t_xs = act.op(lambda: nc.scalar.activation(out=xs2[:], in_=x1[b][:], func=AF.Identity, scale=sm[:, 9:10]),
                                  t_r)
                    x1_free[b] = [t_xs, t_st]
                    pe.after(t_xs, t_a)
                    for dc in range(KC):
                        mm = nc.tensor.transpose(out=pm[:, dc * P:(dc + 1) * P], in_=xs2[:, dc * P:(dc + 1) * P],
                                                 identity=ident_f[:])
                    t_tr = pe.done(mm)
                    t_h2 = None
                    for dc in range(KC):
                        t_h2 = act.op(lambda: nc.scalar.activation(
                            out=h2f[:, dc, :], in_=pm[:, dc * P:(dc + 1) * P], func=AF.Identity,
                            scale=a_f[:, dc:dc + 1], bias=modcol[:, 32 + dc:33 + dc]), t_tr, ser=(dc == 0))
                    t_hb = dve.op(lambda: nc.vector.tensor_copy(out=h2b[b][:], in_=h2f[:]), t_h2, h2b_free[b])
                    h2b_free[b] = dma(sp, ht_send.rearrange("(dc p) t -> p dc t", p=P)[:, :, tt * P:(tt + 1) * P],
                                      h2b[b][:], h2_sl[b], t_hb)
                    pe.after(t_h2, t_wr)
                    for dc in range(KC):
                        mm = nc.tensor.matmul(pm[:, 0:32], lhsT=h2f[:, dc, :], rhs=wr[:, dc, :],
                                              start=(dc == 0), stop=(dc == KC - 1))
                    t_lg = pe.done(mm)
                    t_l = dve.op(lambda: nc.vector.tensor_tensor(out=lg[:], in0=pm[:, 0:32], in1=brb[:], op=ALU.add), t_lg)
                    pm_free[b] = t_l
                    dve.op(lambda: nc.vector.max(out=top8[:], in_=lg[:]))
                    dve.op(lambda: nc.vector.tensor_scalar(out=msk[:], in0=lg[:], scalar1=top8[:, 3:4], scalar2=None,
                                                           op0=ALU.is_ge))
                    t_n = dve.op(lambda: nc.vector.tensor_scalar(out=top8[:, 4:5], in0=top8[:, 0:1], scalar1=-1.0,
                                                                 scalar2=None, op0=ALU.mult))
                    t_e = act.op(lambda: nc.scalar.activation(out=ex[:], in_=lg[:], func=AF.Exp, bias=top8[:, 4:5],
                                                              scale=1.0), t_n)
                    dve.op(lambda: nc.vector.tensor_tensor(out=ex[:], in0=ex[:], in1=msk[:], op=ALU.mult), t_e)
                    dve.op(lambda: nc.vector.tensor_reduce(out=top8[:, 5:6], in_=ex[:], axis=AX.X, op=ALU.add))
                    dve.op(lambda: nc.vector.reciprocal(out=top8[:, 6:7], in_=top8[:, 5:6]))
                    t_g = dve.op(lambda: nc.vector.tensor_scalar(out=gt[b][:], in0=ex[:], scalar1=top8[:, 6:7],
                                                                 scalar2=None, op0=ALU.mult), gt_free[b])
                    gt_free[b] = dma(sp, g_send[tt * P:(tt + 1) * P, :], gt[b][:], gt_sl[b], t_g)
                t_send = [x1_sl[0].tok(), x1_sl[1].tok(), h2_sl[0].tok(), h2_sl[1].tok(), gt_sl[0].tok(), gt_sl[1].tok()]
                t_ph7 = [('e', pe, pe.n), ('e', act, act.n), ('e', dve, dve.n)]
                barrier(*t_send)

        NE = 32
        NU = 16

        def barrier_all(*extra):
            toks = [('e', pe, pe.n), ('e', act, act.n), ('e', dve, dve.n), ('e', pool, pool.n)] + list(extra)
            for en in (pe, act, dve, pool, sp):
                en.after(*toks)

        barrier_all(*t_send)
        with ExitStack() as ph0:
            yacc = sb(ph0, "yacc", [P, 8, D], F32)
            ph = ph0.enter_context(ExitStack())
            hb = sb(ph, "hb", [P, KC, OWN], BF16)
            gb = sb(ph, "gb", [P, 8, 32], F32)
            NS = 2
            NW = 3
            sg_f = [sb(ph, f"sgf{i}", [P, KC, P], F32) for i in range(NS)]
            su_f = [sb(ph, f"suf{i}", [P, KC, P], F32) for i in range(NS)]
            sd_f = [sb(ph, f"sdf{i}", [P, D], F32) for i in range(NS)]
            s_sl = [slot("stg") for _ in range(NS)]
            NGU, NWD, NAV = 2, 5, 4
            wgs = [sb(ph, f"wgs{i}", [P, KC, P], BF16) for i in range(NGU)]
            wus = [sb(ph, f"wus{i}", [P, KC, P], BF16) for i in range(NGU)]
            wds = [sb(ph, f"wds{i}", [P, D], BF16) for i in range(NWD)]
            bdt = sb(ph, "bdt", [P, 512], F32)
            bd_sl = slot("bd")
            bgc = sb(ph, "bgc", [P, NE, KC], F32)
            buc = sb(ph, "buc", [P, NE, KC], F32)
            av = [sb(ph, f"av{i}", [P, OWN], BF16) for i in range(NAV)]
            gcl = [sb(ph, f"gcl{i}", [P, 512], F32) for i in range(2)]
            sg1 = sb(ph, "sg1", [P, 512], F32)
            sg = [sg1, sg1]
            ucl = [sb(ph, f"ucl{i}", [P, 512], F32) for i in range(2)]
            pg = [ps(ph, f"pg{i}", [P, 512]) for i in range(2)]
            pu = [ps(ph, f"pu{i}", [P, 512]) for i in range(2)]
            pd = [ps(ph, f"pd{i}", [P, 512]) for i in range(4)]
            misc = slot("m")
            t_b = dma(sp, bgc[:], bg_col[:, :, :], misc)
            t_b = dma(sp, buc[:], bu_col[:, :, :], misc)
            t_b = dma(sp, hb[:], ht_send.rearrange("(dc p) t -> p dc t", p=P), misc)
            t_b = dma(sp, gb[:], g_send.rearrange("(tt p) e -> p tt e", p=P), misc)
            t_b = dve.op(lambda: nc.vector.tensor_scalar(out=buc[:], in0=buc[:], scalar1=1.0, scalar2=None, op0=ALU.add),
                         t_b)
            wg_v = wg.rearrange("e (kc p) n -> e p kc n", p=P)
            wu_v = wu.rearrange("e (kc p) n -> e p kc n", p=P)
            wd_v = wd.rearrange("e (fc p) n -> e p fc n", p=P)
            st = dict(s_free=[[None, None, None] for _ in range(NS)], gu_free=[None] * NGU, wd_free=[None] * NWD,
                      sg_free=None,
                      pg_free=[None, None], pu_free=[None, None], pd_free=[None] * 4,
                      av_free=[None] * NAV, bd_free=None, dk=0, tmp_free=[None, None])
            units = [(e, un) for e in range(NE) for un in range(NU)]
            PRE = 2

            def issue_w(k):
                e, un = units[k]
                s_ = k % NS
                sp.after(*st['s_free'][s_])
                d1 = s_sl[s_].add(nc.sync.dma_start(out=sg_f[s_][:], in_=wg_v[e][:, :, un * P:(un + 1) * P]))
                d2 = s_sl[s_].add(nc.sync.dma_start(out=su_f[s_][:], in_=wu_v[e][:, :, un * P:(un + 1) * P]))
                d3 = s_sl[s_].add(nc.sync.dma_start(out=sd_f[s_][:], in_=wd_v[e][:, un, :]))
                c1 = act.op(lambda: nc.scalar.copy(out=wgs[k % NGU][:], in_=sg_f[s_][:]), d3, st['gu_free'][k % NGU])
                c2 = act.op(lambda: nc.scalar.copy(out=wus[k % NGU][:], in_=su_f[s_][:]), ser=False)
                c3 = act.op(lambda: nc.scalar.copy(out=wds[k % NWD][:], in_=sd_f[s_][:]), st['wd_free'][k % NWD], ser=False)
                st['s_free'][s_] = [c1, c2, c3]
                return (c1, c2, c3)

            w_tok = {}
            for i in range(PRE):
                w_tok[i] = issue_w(i)

            def pair_tasks(ia, ib):
                for tt in range(8):
                    for db in range(4):
                        yield (ia, ib, tt, db, tt == 7 and db == 3)

            def do_task(task):
                (ia, ib, tt, db, last) = task
                pb = st['dk'] % 4
                st['dk'] += 1
                pe.after(ia['t_avs'][tt // 4], ib['t_avs'][tt // 4], ia['c3'], ib['c3'], st['pd_free'][pb])
                nc.tensor.matmul(pd[pb][:, :], lhsT=av[ia['av']][:, tt * P:(tt + 1) * P],
                                 rhs=wds[ia['wd']][:, db * 512:(db + 1) * 512], start=True, stop=False)
                mm = nc.tensor.matmul(pd[pb][:, :], lhsT=av[ib['av']][:, tt * P:(tt + 1) * P],
                                      rhs=wds[ib['wd']][:, db * 512:(db + 1) * 512], start=False, stop=True)
                t_mm = pe.done(mm)
                e = ia['e']
                t_evl = dve.op(lambda: nc.vector.scalar_tensor_tensor(
                    out=yacc[:, tt, db * 512:(db + 1) * 512], in0=pd[pb][:, :],
                    scalar=gb[:, tt, e:e + 1],
                    in1=yacc[:, tt, db * 512:(db + 1) * 512], op0=ALU.mult, op1=ALU.add), t_mm, ser=False)
                st['pd_free'][pb] = t_evl
                if last:
                    for info in (ia, ib):
                        st['av_free'][info['av']] = t_mm
                        st['wd_free'][info['wd']] = t_mm
                return t_evl

            def run_tasks(gen, n):
                t = None
                for _ in range(n):
                    task = next(gen, None)
                    if task is None:
                        break
                    t = do_task(task)
                return t

            gen = iter(())
            prev = None
            t_dn = None
            for ui, (e, un) in enumerate(units):
                if e == 0:
                    for db_ in (range(4) if un == 0 else []):
                        t_bd = dma(sp, bdt[:], bd_b[e][:, db_ * 512:(db_ + 1) * 512], bd_sl, st['bd_free'])
                        t_i = None
                        for tt in range(8):
                            t_i = dve.op(lambda: nc.vector.tensor_scalar(
                                out=yacc[:, tt, db_ * 512:(db_ + 1) * 512], in0=bdt[:], scalar1=gb[:, tt, e:e + 1],
                                scalar2=None, op0=ALU.mult), t_bd, t_b)
                        st['bd_free'] = t_i
                else:
                    db_ = un // 4
                    if un % 4 == 0:
                        st['t_bd'] = dma(sp, bdt[:], bd_b[e][:, db_ * 512:(db_ + 1) * 512], bd_sl, st['bd_free'])
                    t_i = None
                    for tt in ((un % 4) * 2, (un % 4) * 2 + 1):
                        t_i = dve.op(lambda: nc.vector.scalar_tensor_tensor(
                            out=yacc[:, tt, db_ * 512:(db_ + 1) * 512], in0=bdt[:], scalar=gb[:, tt, e:e + 1],
                            in1=yacc[:, tt, db_ * 512:(db_ + 1) * 512], op0=ALU.mult, op1=ALU.add), st['t_bd'])
                    if un % 4 == 3:
                        st['bd_free'] = t_i
                    dve.after(('e', dve, dve.n))
                gu = ui % NGU
                ab = ui % NAV
                (c1, c2, c3) = w_tok.pop(ui)
                t_avs = []
                cnt = 0
                t_u = None
                for tb in range(2):
                    pe.after(c1, t_b, st['pg_free'][tb])
                    for kc in range(KC):
                        mm = nc.tensor.matmul(pg[tb][:, :], lhsT=wgs[gu][:, kc, :], rhs=hb[:, kc, tb * 512:(tb + 1) * 512],
                                              start=(kc == 0), stop=(kc == KC - 1))
                        if kc == KC - 1:
                            t_g = pe.done(mm)
                        cnt += 1
                        if cnt % 4 == 0:
                            run_tasks(gen, 1)
                    t_gc = dve.op(lambda: nc.vector.tensor_scalar(out=gcl[tb][:], in0=pg[tb][:, :],
                                                                  scalar1=bgc[:, e, un:un + 1], scalar2=LIMIT,
                                                                  op0=ALU.add, op1=ALU.min), t_g, t_b, st['tmp_free'][tb])
                    st['pg_free'][tb] = t_gc
                    t_s = act.op(lambda: nc.scalar.activation(out=sg[tb][:], in_=gcl[tb][:], func=AF.Sigmoid, scale=ALPHA),
                                 t_gc, st['sg_free'])
                    pe.after(c2, st['pu_free'][tb])
                    for kc in range(KC):
                        mm = nc.tensor.matmul(pu[tb][:, :], lhsT=wus[gu][:, kc, :], rhs=hb[:, kc, tb * 512:(tb + 1) * 512],
                                              start=(kc == 0), stop=(kc == KC - 1))
                        if kc == KC - 1:
                            t_u = pe.done(mm)
                        cnt += 1
                        if cnt % 4 == 0:
                            run_tasks(gen, 1)
                    t_ur = act.op(lambda: nc.scalar.activation(out=ucl[tb][:], in_=pu[tb][:, :], func=AF.Identity,
                                                               bias=buc[:, e, un:un + 1], scale=1.0), t_u, st['tmp_free'][tb])
                    st['pu_free'][tb] = t_ur
                    t_gs = pool.op(lambda: nc.gpsimd.tensor_tensor(out=gcl[tb][:], in0=gcl[tb][:], in1=sg[tb][:],
                                                                   op=ALU.mult), t_s)
                    st['sg_free'] = t_gs
                    pool.op(lambda: nc.gpsimd.tensor_scalar(out=ucl[tb][:], in0=ucl[tb][:], scalar1=LIMIT + 1.0,
                                                            scalar2=-LIMIT + 1.0, op0=ALU.min, op1=ALU.max), t_ur)
                    t_av = pool.op(lambda: nc.gpsimd.tensor_tensor(
                        out=av[ab][:, tb * 512:(tb + 1) * 512], in0=ucl[tb][:], in1=gcl[tb][:],
                        op=ALU.mult), st['av_free'][ab] if tb == 0 else None)
                    st['tmp_free'][tb] = t_av
                    t_avs.append(t_av)
                st['gu_free'][gu] = t_u
                info = dict(e=e, av=ab, wd=ui % NWD, t_avs=t_avs, c3=c3)
                if ui % 2 == 1:
                    run_tasks(gen, 64)
                    gen = pair_tasks(prev, info)
                else:
                    prev = info
                if ui + PRE < len(units):
                    w_tok[ui + PRE] = issue_w(ui + PRE)
            t_dn = run_tasks(gen, 64)
            barrier_all()
            ph.close()
            ph = ph0

            xr = [sb(ph, f"xr{i}", [P, D], F32) for i in range(2)]
            ld_sl = [slot("fl") for _ in range(2)]
            st_sl = [slot("fs") for _ in range(2)]
            junk = sb(ph, "junk9", [P, D], F32)
            fs = sb(ph, "fs", [P, 16], F32)
            gf_row = sb(ph, "gf_row10", [P, D], F32)
            t_gf = dma(sp, gf_row[:], gf_scr[:, :], slot("m"))
            dve.op(lambda: nc.vector.memset(fs[:], 0.0))
            st_free = [None, None]
            for tt in range(8):
                b = tt % 2
                t_ld = dma(sp, xr[b][:], x1_scr[tt * P:(tt + 1) * P, :], ld_sl[b], st_free[b])
                t_sq = act.op(lambda: nc.scalar.activation(out=junk[:], in_=yacc[:, tt, :], func=AF.Square,
                                                           accum_out=fs[:, tt:tt + 1]), t_dn)
                rstd(fs[:, 8 + tt:9 + tt], fs[:, tt:tt + 1], 1.0 / D, t_sq)
                dve.op(lambda: nc.vector.scalar_tensor_tensor(out=yacc[:, tt, :], in0=yacc[:, tt, :],
                                                              scalar=fs[:, 8 + tt:9 + tt],
                                                              in1=gf_row[:], op0=ALU.mult, op1=ALU.mult), t_gf)
                t_o = dve.op(lambda: nc.vector.tensor_tensor(out=xr[b][:], in0=yacc[:, tt, :], in1=xr[b][:], op=ALU.add),
                             t_ld)
                st_free[b] = dma(sp, out[tt * P:(tt + 1) * P, :], xr[b][:], st_sl[b], t_o)
            sp.after(st_sl[0].tok(), st_sl[1].tok())
    return nc


_CACHE = {}


def _bf(a):
    return np.ascontiguousarray(a.astype(ml_dtypes.bfloat16))


def _f(a):
    return np.ascontiguousarray(a, dtype=np.float32)


def _col(v):
    return _f(np.asarray(v).reshape(KC, P).T)


def _rep(v):
    return _f(np.broadcast_to(np.asarray(v)[None, :], (P, v.shape[-1])))


def kernel(x, c, w_ada, b_ada, g_pre_mix, w_in, w_fourier, q_norm_g, k_norm_g, g_fourier_out, g_attn_out, w_out,
           g_post_mix, g_pre_ffn, w_router, b_router, w_gate, b_gate, w_up, b_up, w_down, b_down, g_post_ffn):
    arrs = [np.asarray(a) for a in (x, c, w_ada, b_ada, g_pre_mix, w_in, w_fourier, q_norm_g, k_norm_g,
                                    g_fourier_out, g_attn_out, w_out, g_post_mix, g_pre_ffn, w_router, b_router,
                                    w_gate, b_gate, w_up, b_up, w_down, b_down, g_post_ffn)]
    (x, c, w_ada, b_ada, g_pre_mix, w_in, w_fourier, q_norm_g, k_norm_g, g_fourier_out, g_attn_out, w_out,
     g_post_mix, g_pre_ffn, w_router, b_router, w_gate, b_gate, w_up, b_up, w_down, b_down, g_post_ffn) = arrs
    if 'nc' not in _CACHE:
        _CACHE['nc'] = build()
    nc = _CACHE['nc']
    inv_freq = (1.0 / (10000.0 ** (np.arange(0, 64, 2, dtype=np.float32) / 64.0))).astype(np.float32)
    cc64 = np.arange(128, dtype=np.int64)
    ang_c = 2.0 * np.pi * ((cc64[:, None] * cc64[None, :]) % 128).astype(np.float64) / 128.0
    cs_tab = _f(np.concatenate([np.cos(ang_c) / 512.0, -np.sin(ang_c) / 512.0], axis=1))
    ident = np.eye(P, dtype=np.float32)
    shared = dict(
        w_ada=_f(w_ada[0]), b_ada=_f(b_ada[0][None, :]), gpm_col=_col(g_pre_mix[0]), gpf_col=_col(g_pre_ffn[0]),
        w_in=_f(w_in[0]), w_out=_f(w_out[0]), wf=_f(np.transpose(w_fourier[0], (1, 0, 2))),
        qg_b=_rep(q_norm_g[0]), kg_b=_rep(k_norm_g[0]),
        gmerge_b=_rep(np.concatenate([g_fourier_out[0], g_attn_out[0]])),
        gpostm_b=_rep(g_post_mix[0]), gpostf_b=_rep(g_post_ffn[0]),
        w_router=_f(np.transpose(w_router[0].reshape(KC, P, 32), (1, 0, 2))), b_router_b=_rep(b_router[0]),
        cs_tab=cs_tab, ident_in=ident,
        wg=_f(w_gate[0]), wu=_f(w_up[0]), wd=_f(w_down[0]),
        bg_col=_f(np.transpose(b_gate[0].reshape(32, KC, P), (2, 0, 1))),
        bu_col=_f(np.transpose(b_up[0].reshape(32, KC, P), (2, 0, 1))),
        bd_b=_f(np.broadcast_to(b_down[0][:, None, :], (32, P, D))),
    )
    in_maps = []
    for core in range(NCORE):
        b, half = core // 2, core % 2
        own = np.arange(half * OWN, (half + 1) * OWN)
        oth = np.arange((1 - half) * OWN, (2 - half) * OWN)
        perm = np.concatenate([own, oth])
        pos = perm.astype(np.float32)
        row_idx = np.floor(pos / 64.0).astype(np.float32)
        col_idx = (pos - row_idx * 64.0).astype(np.float32)
        ang = np.concatenate([row_idx[:, None] * inv_freq[None, :], col_idx[:, None] * inv_freq[None, :]],
                             axis=1).astype(np.float32)
        cos_t = np.cos(ang).astype(np.float32).reshape(16, P, 64).transpose(1, 0, 2)
        sin_t = np.sin(ang).astype(np.float32).reshape(16, P, 64).transpose(1, 0, 2)
        prod = (perm.astype(np.int64)[:, None] * own.astype(np.int64)[None, :]) % S
        ang_s = 2.0 * np.pi * prod.astype(np.float64) / S
        m = dict(shared)
        m.update(
            xp=_f(x[b][perm]), c_col=_col(c[b]),
            cos_t=_f(cos_t), sin_t=_f(sin_t), ctab=_f(np.cos(ang_s)), stab=_f(np.sin(ang_s)),
        )
        in_maps.append(m)
    res = run_bass_kernel_spmd(nc, in_maps, core_ids=list(range(NCORE)))
    outp = np.empty((4, S, D), np.float32)
    for core in range(NCORE):
        b, half = core // 2, core % 2
        outp[b, half * OWN:(half + 1) * OWN] = res.results[core]["out"]
    return outp
```
